# Optimizing a Trainium2 kernel written in Bass

```python
import math
import jax, jax.numpy as jnp
from jax import lax
import numpy as np

D_MODEL = 1024
BATCH = 8
SEQ = 4096
DEPTH = 2

HEAD_DIM = 64
MOBA_HEADS = (D_MODEL // 2) // HEAD_DIM
MOBA_BLOCK = 256
MOBA_TOPK = 3
MOBA_QCHUNK = 32
DIFF_QK_DIM = HEAD_DIM
DIFF_V_DIM = 2 * DIFF_QK_DIM
DIFF_HEADS = (D_MODEL // 2) // DIFF_V_DIM
DIFF_QBLOCK = 128
D_FF = ((8 * D_MODEL // 3 + 127) // 128) * 128
RMS_EPS = 1e-6
MOBA_W = MOBA_HEADS * HEAD_DIM
DIFF_QK_W = DIFF_HEADS * 2 * DIFF_QK_DIM
DIFF_V_W = DIFF_HEADS * DIFF_V_DIM
MIX_W = MOBA_W + DIFF_V_W
IN_W = 3 * MOBA_W + 2 * DIFF_QK_W + DIFF_V_W
SPLITS = tuple(np.cumsum([MOBA_W, MOBA_W, MOBA_W, DIFF_QK_W, DIFF_QK_W]).tolist())

kernel_name = "hymba_moba_diffattn_macaron_sandwich"


def alibi_slopes(n):
    return jnp.asarray(2.0 ** (-8.0 * np.arange(1, n + 1) / n), dtype=jnp.float32)


def rmsnorm(x, g):
    xf = x.astype(jnp.float32)
    y = xf * lax.rsqrt(jnp.mean(xf * xf, axis=-1, keepdims=True) + RMS_EPS)
    return (y * g.astype(jnp.float32)).astype(x.dtype)


def swiglu(h, w_gate, w_up, w_down):
    return (jax.nn.silu(h @ w_gate) * (h @ w_up)) @ w_down


def moba_attention(q, k, v, slopes):
    B, H, S, dh = q.shape
    nb = -(-S // MOBA_BLOCK)
    sp = nb * MOBA_BLOCK
    pad = sp - S
    if pad:
        q, k, v = [jnp.pad(t, ((0, 0), (0, 0), (0, pad), (0, 0))) for t in (q, k, v)]
    kb = k.reshape(B, H, nb, MOBA_BLOCK, dh)
    vb = v.reshape(B, H, nb, MOBA_BLOCK, dh)
    kmean = jnp.mean(kb.astype(jnp.float32), axis=3)
    ksel = min(MOBA_TOPK, nb)
    scale = dh ** -0.5
    nch = sp // MOBA_QCHUNK
    qc = q.reshape(B, H, nch, MOBA_QCHUNK, dh).transpose(2, 0, 1, 3, 4)
    gather = jax.vmap(jax.vmap(lambda blocks, idx: blocks[idx]))
    offs = jnp.arange(MOBA_BLOCK)
    blk_ids = jnp.arange(nb)
    sel_ids = jnp.arange(ksel)

    def chunk(args):
        ci, qq = args
        qpos = ci * MOBA_QCHUNK + jnp.arange(MOBA_QCHUNK)
        cur = (ci * MOBA_QCHUNK) // MOBA_BLOCK
        qf = qq.astype(jnp.float32)
        gate = jnp.einsum('bhqd,bhnd->bhqn', qf, kmean)
        gate = jnp.where(blk_ids < cur, gate, -jnp.inf)
        _, idx = lax.top_k(gate, ksel)
        valid = sel_ids < cur
        k_sel = gather(kb, idx).astype(jnp.float32)
        v_sel = gather(vb, idx).astype(jnp.float32)
        s_sel = jnp.einsum('bhqd,bhqjkd->bhqjk', qf, k_sel) * scale
        kpos_sel = idx[..., None] * MOBA_BLOCK + offs
        s_sel = s_sel - slopes[:, None, None, None] * (qpos[:, None, None] - kpos_sel)
        s_sel = jnp.where(valid[:, None], s_sel, -jnp.inf)
        s_sel = s_sel.reshape(B, H, MOBA_QCHUNK, ksel * MOBA_BLOCK)
        k_own = lax.dynamic_slice_in_dim(kb, cur, 1, axis=2)[:, :, 0].astype(jnp.float32)
        v_own = lax.dynamic_slice_in_dim(vb, cur, 1, axis=2)[:, :, 0].astype(jnp.float32)
        kpos_own = cur * MOBA_BLOCK + offs
        dist = qpos[:, None] - kpos_own[None, :]
        s_own = jnp.einsum('bhqd,bhkd->bhqk', qf, k_own) * scale - slopes[:, None, None] * dist
        s_own = jnp.where(dist >= 0, s_own, -jnp.inf)
        p = jax.nn.softmax(jnp.concatenate([s_sel, s_own], axis=-1), axis=-1)
        p_sel = p[..., :ksel * MOBA_BLOCK].reshape(B, H, MOBA_QCHUNK, ksel, MOBA_BLOCK)
        p_own = p[..., ksel * MOBA_BLOCK:]
        o = (jnp.einsum('bhqjk,bhqjkd->bhqd', p_sel, v_sel)
             + jnp.einsum('bhqk,bhkd->bhqd', p_own, v_own))
        return o.astype(q.dtype)

    out = lax.map(chunk, (jnp.arange(nch), qc))
    out = out.transpose(1, 2, 0, 3, 4).reshape(B, H, sp, dh)
    return out[:, :, :S]


def diff_attention(q, k, v, lam, lam_init, subln_g, slopes):
    B, H, _, S, d = q.shape
    nqb = S // DIFF_QBLOCK
    scale = d ** -0.5
    qb = q.reshape(B, H, 2, nqb, DIFF_QBLOCK, d).transpose(3, 0, 1, 2, 4, 5)
    kf = k.astype(jnp.float32)
    vf = v.astype(jnp.float32)
    kpos = jnp.arange(S)

    def blk(args):
        bi, qq = args
        qpos = bi * DIFF_QBLOCK + jnp.arange(DIFF_QBLOCK)
        dist = qpos[:, None] - kpos[None, :]
        s = jnp.einsum('bhmqd,bhmkd->bhmqk', qq.astype(jnp.float32), kf) * scale
        s = s - slopes[:, None, None, None] * dist
        s = jnp.where(dist >= 0, s, -jnp.inf)
        p = jax.nn.softmax(s, axis=-1)
        a = p[:, :, 0] - lam * p[:, :, 1]
        return jnp.einsum('bhqk,bhkd->bhqd', a, vf)

    out = lax.map(blk, (jnp.arange(nqb), qb))
    out = out.transpose(1, 2, 0, 3, 4).reshape(B, H, S, 2 * d)
    out = rmsnorm(out, subln_g) * (1.0 - lam_init)
    return out.astype(q.dtype)


def setup_inputs(seed: int = 0) -> dict:
    key = jax.random.key(seed)
    ks = jax.random.split(key, 24)
    L = DEPTH

    def w(k, shape, fan_in):
        return jax.random.normal(k, shape, jnp.float32) * (fan_in ** -0.5)

    def gain(k, shape):
        return 1.0 + 0.02 * jax.random.normal(k, shape, jnp.float32)

    return {
        "x": jax.random.normal(ks[0], (BATCH, SEQ, D_MODEL), jnp.float32),
        "ffn1_pre_g": gain(ks[1], (L, D_MODEL)),
        "ffn1_w_gate": w(ks[2], (L, D_MODEL, D_FF), D_MODEL),
        "ffn1_w_up": w(ks[3], (L, D_MODEL, D_FF), D_MODEL),
        "ffn1_w_down": w(ks[4], (L, D_FF, D_MODEL), D_FF),
        "ffn1_post_g": gain(ks[5], (L, D_MODEL)),
        "mix_pre_g": gain(ks[6], (L, D_MODEL)),
        "w_in": w(ks[7], (L, D_MODEL, IN_W), D_MODEL),
        "lambda_q1": 0.1 * jax.random.normal(ks[8], (L, DIFF_QK_DIM), jnp.float32),
        "lambda_k1": 0.1 * jax.random.normal(ks[9], (L, DIFF_QK_DIM), jnp.float32),
        "lambda_q2": 0.1 * jax.random.normal(ks[10], (L, DIFF_QK_DIM), jnp.float32),
        "lambda_k2": 0.1 * jax.random.normal(ks[11], (L, DIFF_QK_DIM), jnp.float32),
        "subln_g": gain(ks[12], (L, DIFF_V_DIM)),
        "w_out": w(ks[13], (L, MIX_W, D_MODEL), MIX_W),
        "mix_post_g": gain(ks[14], (L, D_MODEL)),
        "ffn2_pre_g": gain(ks[15], (L, D_MODEL)),
        "ffn2_w_gate": w(ks[16], (L, D_MODEL, D_FF), D_MODEL),
        "ffn2_w_up": w(ks[17], (L, D_MODEL, D_FF), D_MODEL),
        "ffn2_w_down": w(ks[18], (L, D_FF, D_MODEL), D_FF),
        "ffn2_post_g": gain(ks[19], (L, D_MODEL)),
    }


def reference(x, ffn1_pre_g, ffn1_w_gate, ffn1_w_up, ffn1_w_down, ffn1_post_g,
              mix_pre_g, w_in, lambda_q1, lambda_k1, lambda_q2, lambda_k2, subln_g,
              w_out, mix_post_g, ffn2_pre_g, ffn2_w_gate, ffn2_w_up, ffn2_w_down, ffn2_post_g):
    B, S, _ = x.shape
    slopes_moba = alibi_slopes(MOBA_HEADS)
    slopes_diff = alibi_slopes(DIFF_HEADS)
    for l in range(DEPTH):
        h = rmsnorm(x, ffn1_pre_g[l])
        x = x + 0.5 * rmsnorm(swiglu(h, ffn1_w_gate[l], ffn1_w_up[l], ffn1_w_down[l]), ffn1_post_g[l])

        h = rmsnorm(x, mix_pre_g[l])
        proj = h @ w_in[l]
        q_m, k_m, v_m, q_d, k_d, v_d = jnp.split(proj, SPLITS, axis=-1)
        to_heads = lambda t: t.reshape(B, S, MOBA_HEADS, HEAD_DIM).transpose(0, 2, 1, 3)
        o_m = moba_attention(to_heads(q_m), to_heads(k_m), to_heads(v_m), slopes_moba)
        o_m = o_m.transpose(0, 2, 1, 3).reshape(B, S, MOBA_W)

        to_pairs = lambda t: t.reshape(B, S, DIFF_HEADS, 2, DIFF_QK_DIM).transpose(0, 2, 3, 1, 4)
        v_dh = v_d.reshape(B, S, DIFF_HEADS, DIFF_V_DIM).transpose(0, 2, 1, 3)
        lam_init = 0.8 - 0.6 * math.exp(-0.3 * l)
        lam = (jnp.exp(jnp.sum(lambda_q1[l].astype(jnp.float32) * lambda_k1[l].astype(jnp.float32)))
               - jnp.exp(jnp.sum(lambda_q2[l].astype(jnp.float32) * lambda_k2[l].astype(jnp.float32)))
               + lam_init)
        o_d = diff_attention(to_pairs(q_d), to_pairs(k_d), v_dh, lam, lam_init, subln_g[l], slopes_diff)
        o_d = o_d.transpose(0, 2, 1, 3).reshape(B, S, DIFF_V_W)

        mix = jnp.concatenate([o_m, o_d], axis=-1)
        x = x + rmsnorm(mix @ w_out[l], mix_post_g[l])

        h = rmsnorm(x, ffn2_pre_g[l])
        x = x + 0.5 * rmsnorm(swiglu(h, ffn2_w_gate[l], ffn2_w_up[l], ffn2_w_down[l]), ffn2_post_g[l])
    return x
```

```python
import math
import contextlib
import numpy as np
import concourse.bass as bass
import concourse.mybir as mybir
from concourse.bass_utils import run_bass_kernel_spmd

F32 = mybir.dt.float32
BF16 = mybir.dt.bfloat16
AF = mybir.ActivationFunctionType
ALU = mybir.AluOpType
AX = mybir.AxisListType

D = 1024
S = 4096
L = 2
FF = 2816
NFC = FF // 128
HD = 64
MH = 8
DH = 4
INW = 3072
EPS = 1e-6
TT = 512
NT = S // TT
NEG = -30000.0


class Res:
    __slots__ = ("name", "last_write", "reads")

    def __init__(self, name):
        self.name = name
        self.last_write = None
        self.reads = []


class Op:
    __slots__ = ("eng", "fn", "seq", "waits", "signal", "sig", "is_dma", "dsem", "dval", "dprev")

    def __init__(self, eng, fn, is_dma=False):
        self.eng = eng
        self.fn = fn
        self.seq = 0
        self.waits = {}
        self.signal = False
        self.sig = 0
        self.is_dma = is_dma
        self.dsem = None
        self.dval = 0
        self.dprev = None


class Prog:
    ENGS = ("pe", "act", "dve", "pool", "sp")

    def __init__(self, nc, ndma_sems=12):
        self.nc = nc
        self.ops = {e: [] for e in self.ENGS}
        self.known = {e: {} for e in self.ENGS}
        self.dma_count = {e: 0 for e in self.ENGS}
        self.dma_ops = {e: [] for e in self.ENGS}
        self.ndma = ndma_sems
        self.res = {}
        self.ambient = []

    def R(self, name):
        r = self.res.get(name)
        if r is None:
            r = self.res[name] = Res(name)
        return r

    def _dep(self, a, b):
        if a is None or a is b:
            return
        if a.is_dma:
            key = ("dma",) + a.dsem
            if self.known[b.eng].get(key, 0) >= a.dval:
                return
            self.known[b.eng][key] = a.dval
            b.waits[key] = a
            return
        if a.eng == b.eng and a.eng == "pe":
            return
        k = self.known[b.eng].get(a.eng, 0)
        if a.seq <= k:
            return
        cur = b.waits.get(a.eng)
        if cur is None or cur.seq < a.seq:
            b.waits[a.eng] = a
        a.signal = True

    def op(self, eng, fn, reads=(), writes=(), is_dma=False):
        o = Op(eng, fn, is_dma)
        lst = self.ops[eng]
        lst.append(o)
        o.seq = len(lst)
        writes = list(writes) + [r for r in reads if isinstance(r, str) and r.startswith("bank")]
        reads = [r for r in reads if not (isinstance(r, str) and r.startswith("bank"))]
        reads = [self.R(r) if isinstance(r, str) else r for r in list(reads) + self.ambient]
        writes = [self.R(w) if isinstance(w, str) else w for w in writes]
        for r in reads:
            self._dep(r.last_write, o)
        for w in writes:
            self._dep(w.last_write, o)
            for rd in w.reads:
                self._dep(rd, o)
        for r in reads:
            r.reads.append(o)
        for w in writes:
            w.last_write = o
            w.reads = []
        for k, a in o.waits.items():
            if not isinstance(k, tuple):
                self.known[eng][k] = max(self.known[eng].get(k, 0), a.seq)
        if is_dma:
            i = self.dma_count[eng]
            self.dma_count[eng] = i + 1
            o.dsem = (eng, i % self.ndma)
            o.dval = 16 * (i // self.ndma + 1)
            if i >= self.ndma:
                o.dprev = self.dma_ops[eng][i - self.ndma]
            self.dma_ops[eng].append(o)
        return o

    def dma(self, eng, out, in_, reads=(), writes=(), **kw):
        return self.op(eng, lambda e: e.dma_start(out=out, in_=in_, **kw), reads, writes, is_dma=True)

    def emit(self, final_waits=()):
        nc = self.nc
        engobj = {"pe": "tensor", "act": "scalar", "dve": "vector", "pool": "gpsimd", "sp": "sync"}
        with contextlib.ExitStack() as st:
            sems = {e: st.enter_context(nc.semaphore("s_" + e)) for e in self.ENGS}
            dsems = {}
            for e in self.ENGS:
                for k in range(min(self.ndma, self.dma_count[e])):
                    dsems[(e, k)] = st.enter_context(nc.semaphore("d_%s_%d" % (e, k)))
            for e in self.ENGS:
                c = 0
                for o in self.ops[e]:
                    if o.signal and not o.is_dma:
                        c += 1
                        o.sig = c
            block = st.enter_context(nc.Block())

            def run(e):
                def body(eng):
                    for o in self.ops[e]:
                        if o.dprev is not None:
                            eng.wait_ge(dsems[o.dprev.dsem], o.dprev.dval)
                        for k, a in o.waits.items():
                            if a.is_dma:
                                eng.wait_ge(dsems[a.dsem], a.dval)
                            else:
                                eng.wait_ge(sems[a.eng], a.sig)
                        ins = o.fn(eng)
                        if o.is_dma:
                            ins.then_inc(dsems[o.dsem], 16)
                        elif o.signal:
                            ins.then_inc(sems[e], 1)
                    if e == "sp":
                        for a in final_waits:
                            eng.wait_ge(dsems[a.dsem], a.dval)
                return body

            for e in self.ENGS:
                getattr(block, engobj[e])(run(e))


SB_BASE = 16512
SB_END = 229376


class Builder:
    def __init__(self, stages):
        self.stages = stages
        nc = self.nc = bass.Bass("TRN2", target_bir_lowering=False)
        self.P = Prog(nc)
        self.sb_off = SB_BASE
        self.uid = 0
        dt = lambda name, shape, dtype=F32, kind="ExternalInput": nc.dram_tensor(name, shape, dtype, kind=kind).ap()
        self.x_in = dt("x", [S, D])
        self.prm = {}
        for name, shape in [
            ("ffn1_pre_g", [L, D]), ("ffn1_w_gate", [L, D, FF]), ("ffn1_w_up", [L, D, FF]), ("ffn1_w_down", [L, FF, D]),
            ("ffn1_post_g", [L, D]), ("mix_pre_g", [L, D]), ("w_in", [L, D, INW]),
            ("lambda_q1", [L, HD]), ("lambda_k1", [L, HD]), ("lambda_q2", [L, HD]), ("lambda_k2", [L, HD]),
            ("subln_g", [L, 128]), ("w_out", [L, D, D]), ("mix_post_g", [L, D]),
            ("ffn2_pre_g", [L, D]), ("ffn2_w_gate", [L, D, FF]), ("ffn2_w_up", [L, D, FF]), ("ffn2_w_down", [L, FF, D]),
            ("ffn2_post_g", [L, D]),
        ]:
            self.prm[name] = dt(name, shape)
        self.out = dt("out", [S, D], F32, "ExternalOutput")
        self.xs = dt("xs", [S, D], F32, "Internal")
        self.bank = [nc.alloc_psum_tensor("bank%d" % i, [128, 512], F32) for i in range(8)]
        self.out_dmas = []
        self.QA = dt("QA", [MH, 82, S], BF16, "Internal")
        self.KA = dt("KA", [MH, 82, S], BF16, "Internal")
        self.VM = dt("VM", [S, MH * 65], BF16, "Internal")
        self.QD = dt("QD", [DH, 2, 66, S], BF16, "Internal")
        self.KD = dt("KD", [DH, 2, 66, S], BF16, "Internal")
        self.VD = dt("VD", [S, DH * 129], BF16, "Internal")
        self.MIX = dt("MIX", [S, D], BF16, "Internal")

    def sb(self, name, shape, dtype, off=None):
        esz = 2 if dtype == BF16 else 4
        n = esz
        for s_ in shape[1:]:
            n *= s_
        n = (n + 31) // 32 * 32
        if off is None:
            off = self.sb_off
            self.sb_off += n
            assert self.sb_off <= SB_END, (name, self.sb_off)
        self.uid += 1
        return self.nc.alloc_sbuf_tensor_at("%s_%d" % (name, self.uid), shape, dtype, offset=off)

    def build(self):
        P = self.P
        nc = self.nc
        self.wg = self.sb("wg", [128, 8, FF], BF16)
        self.wu = self.sb("wu", [128, 8, FF], BF16)
        self.wd = self.sb("wd", [128, NFC, D], BF16)
        self.ident = self.sb("ident", [128, 128], BF16)
        self.identf = self.sb("identf", [128, 128], F32)
        self.stat = self.sb("stat", [128, 64], F32)
        self.regB = self.sb_off
        self.eps = self.stat[:, 63:64]
        P.op("pool", lambda e: e.memset(self.eps, EPS), writes=["eps"])
        P.ambient = ["regB"]
        onesf = self.identf
        P.op("pool", lambda e: e.memset(onesf[:], 1.0), writes=["identf"])
        P.op("pool", lambda e: e.affine_select(out=onesf[:], in_=onesf[:], pattern=[[-1, 128]], compare_op=ALU.is_equal,
                                               fill=0.0, base=0, channel_multiplier=1), reads=["identf"], writes=["identf"])
        P.op("pool", lambda e: e.tensor_copy(out=self.ident[:], in_=onesf[:]), reads=["identf"], writes=["ident"])
        if self.stages > 1:
            self.attn_consts()

        src = self.x_in
        nst = 0
        for l in range(L):
            for which in ("ffn1", "attn", "ffn2"):
                if nst >= self.stages:
                    break
                nst += 1
                last = (nst == self.stages) or (l == L - 1 and which == "ffn2")
                dst = self.out if last else self.xs
                if which == "attn":
                    self.attn_phase(l, src, dst)
                elif DBG < 41:
                    self.ffn_phase(l, which, src, dst)
                src = self.xs
        P.emit(final_waits=self.out_dmas)
        return nc

    def rows(self, t_ap, r0, n=128):
        return t_ap[r0:r0 + n, :]

    def ffn_phase(self, l, which, src, dst):
        P = self.P
        nc = self.nc
        is_out = dst is self.out
        pre_g = self.prm[which + "_pre_g"]
        post_g = self.prm[which + "_post_g"]
        wgd = self.prm[which + "_w_gate"][l].rearrange("(kc p) f -> p kc f", p=128)
        wud = self.prm[which + "_w_up"][l].rearrange("(kc p) f -> p kc f", p=128)
        wdd = self.prm[which + "_w_down"][l].rearrange("(fc p) d -> p fc d", p=128)
        self.barrier()
        self.sb_off = self.regB
        gpre = self.sb("gpre", [128, D], F32)
        gpost = self.sb("gpost", [128, D], F32)
        xst = [self.sb("xst", [128, D], F32) for _ in range(2)]
        hst = [self.sb("hst", [128, D], BF16) for _ in range(2)]
        hT = [self.sb("hT", [128, 8, TT], BF16) for _ in range(2)]
        sg = [self.sb("sg", [128, TT], BF16) for _ in range(2)]
        uT = self.sb("uT", [128, NFC, TT], BF16)
        junk = self.sb("junk", [128, D], BF16)
        tst = [self.sb("tst", [128, D], F32) for _ in range(2)]
        stat = self.stat
        tag = "%s%d" % (which, l)
        bank = self.bank

        P.dma("pool", gpre[:], pre_g[l].partition_broadcast(128), writes=["gpre"])
        P.dma("pool", gpost[:], post_g[l].partition_broadcast(128), writes=["gpost"])
        if self.w_loaded != (l, which):
            self.ffn_weights(l, which)
        P.op("pool", lambda e: e.tensor_scalar(out=gpost[:], in0=gpost[:], scalar1=0.5, scalar2=None, op0=ALU.mult),
             reads=["gpost"], writes=["gpost"])

        def prologue(t):
            hTt = hT[t % 2]
            for sub in range(4):
                r0 = t * TT + sub * 128
                xb = xst[sub % 2]
                hb = hst[sub % 2]
                xr, hr = "xst%d" % (sub % 2), "hst%d" % (sub % 2)
                P.dma("sp", xb[:], src[r0:r0 + 128, :], reads=["xs_r%d" % (r0 // 128)], writes=[xr])
                sc = (t * 4 + sub) % 8
                ss = stat[:, sc:sc + 1]
                P.op("act", lambda e, xb=xb, ss=ss: e.activation(out=junk[:], in_=xb[:], func=AF.Square, accum_out=ss),
                     reads=[xr], writes=["junk", "ss%d" % sc])
                if DBG == 21:
                    continue
                P.op("act", lambda e, ss=ss: e.activation(out=ss, in_=ss, func=AF.Sqrt, scale=1.0 / D, bias=self.eps),
                     reads=["eps", "ss%d" % sc], writes=["ss%d" % sc])
                P.op("dve", lambda e, ss=ss: e.reciprocal(out=ss, in_=ss), reads=["ss%d" % sc], writes=["ss%d" % sc])
                P.op("dve", lambda e, xb=xb, hb=hb, ss=ss: e.scalar_tensor_tensor(
                    out=hb[:], in0=xb[:], scalar=ss, in1=gpre[:], op0=ALU.mult, op1=ALU.mult),
                    reads=[xr, "ss%d" % sc, "gpre"], writes=[hr])
                if DBG == 22:
                    continue
                for c in range(8):
                    bk = 4 + c // 2
                    pv = bank[bk][:, :].bitcast(BF16)
                    col = (c % 2) * 512 + sub * 128
                    P.op("pe", lambda e, pv=pv, col=col, hb=hb, c=c: e.transpose(
                        out=pv[:, col:col + 128], in_=hb[:, c * 128:(c + 1) * 128], identity=self.ident[:]),
                        reads=[hr, "ident"], writes=["bank%d" % bk])
            if DBG in (21, 22, 23):
                return
            for c in range(8):
                bk = 4 + c // 2
                pv = bank[bk][:, :].bitcast(BF16)
                eng = "act" if bk % 2 == 0 else "dve"
                if eng == "act":
                    fn = lambda e, pv=pv, c=c: e.activation(out=hTt[:, c, :], in_=pv[:, (c % 2) * 512:(c % 2) * 512 + 512], func=AF.Copy)
                else:
                    fn = lambda e, pv=pv, c=c: e.tensor_copy(out=hTt[:, c, :], in_=pv[:, (c % 2) * 512:(c % 2) * 512 + 512])
                P.op(eng, fn, reads=["bank%d" % bk], writes=["hT%d_%d" % (t % 2, c)])

        def gate_up(t, c):
            hTt = hT[t % 2]
            bg, bu = (0, 1) if c % 2 == 0 else (2, 3)
            for kc in range(8):
                P.op("pe", lambda e, kc=kc: e.matmul(bank[bg][:, :], lhsT=self.wg[:, kc, c * 128:(c + 1) * 128],
                                                     rhs=hTt[:, kc, :], start=(kc == 0), stop=(kc == 7)),
                     reads=["wg%d" % c, "hT%d_%d" % (t % 2, kc)], writes=["bank%d" % bg])
            for kc in range(8):
                P.op("pe", lambda e, kc=kc: e.matmul(bank[bu][:, :], lhsT=self.wu[:, kc, c * 128:(c + 1) * 128],
                                                     rhs=hTt[:, kc, :], start=(kc == 0), stop=(kc == 7)),
                     reads=["wu%d" % c, "hT%d_%d" % (t % 2, kc)], writes=["bank%d" % bu])
            sgb = sg[c % 2]
            P.op("act", lambda e: e.activation(out=sgb[:], in_=bank[bg][:, :], func=AF.Silu),
                 reads=["bank%d" % bg], writes=["sg%d" % (c % 2)])
            P.op("dve", lambda e: e.tensor_tensor(out=uT[:, c, :], in0=bank[bu][:, :], in1=sgb[:], op=ALU.mult),
                 reads=["bank%d" % bu, "sg%d" % (c % 2)], writes=["uT%d" % c])

        def down(t, sub):
            b0 = 4 + 2 * (sub % 2)
            r0 = t * TT + sub * 128
            for half in range(2):
                bk = b0 + half
                for c in range(NFC):
                    P.op("pe", lambda e, c=c, bk=bk, half=half: e.matmul(
                        bank[bk][:, :], lhsT=uT[:, c, sub * 128:(sub + 1) * 128],
                        rhs=self.wd[:, c, half * 512:(half + 1) * 512], start=(c == 0), stop=(c == NFC - 1)),
                        reads=["uT%d" % c, "wd%d" % c], writes=["bank%d" % bk])
            sc = 8 + (t * 4 + sub) % 8
            ss2 = [stat[:, sc + 8 * hf:sc + 8 * hf + 1] for hf in range(2)]
            ss = stat[:, sc:sc + 1]
            for hf in range(2):
                P.op("act", lambda e, hf=hf: e.activation(out=junk[:, hf * 512:(hf + 1) * 512], in_=bank[b0 + hf][:, :],
                                                        func=AF.Square, accum_out=ss2[hf]),
                     reads=["bank%d" % (b0 + hf)], writes=["junk", "ssy%d_%d" % (sc, hf)])
            P.op("dve", lambda e: e.tensor_tensor(out=ss, in0=ss2[0], in1=ss2[1], op=ALU.add),
                 reads=["ssy%d_0" % sc, "ssy%d_1" % sc], writes=["ssy%d_0" % sc])
            P.op("act", lambda e: e.activation(out=ss, in_=ss, func=AF.Sqrt, scale=1.0 / D, bias=self.eps),
                 reads=["eps", "ssy%d_0" % sc], writes=["ssy%d_0" % sc])
            P.op("dve", lambda e: e.reciprocal(out=ss, in_=ss), reads=["ssy%d_0" % sc], writes=["ssy%d_0" % sc])
            tb = tst[sub % 2]
            xb = xst[sub % 2]
            tr, xr = "tst%d" % (sub % 2), "xst%d" % (sub % 2)
            P.dma("sp", xb[:], src[r0:r0 + 128, :], reads=["xs_r%d" % (r0 // 128)], writes=[xr])
            for hf in range(2):
                P.op("dve", lambda e, hf=hf: e.scalar_tensor_tensor(
                    out=tb[:, hf * 512:(hf + 1) * 512], in0=bank[b0 + hf][:, :], scalar=ss,
                    in1=gpost[:, hf * 512:(hf + 1) * 512], op0=ALU.mult, op1=ALU.mult),
                    reads=["bank%d" % (b0 + hf), "ssy%d_0" % sc, "gpost"], writes=[tr])
            P.op("dve", lambda e: e.tensor_tensor(out=tb[:], in0=tb[:], in1=xb[:], op=ALU.add),
                 reads=[tr, xr], writes=[tr])
            o = P.dma("sp", dst[r0:r0 + 128, :], tb[:], reads=[tr], writes=["xs_r%d" % (r0 // 128)])
            if is_out:
                self.out_dmas.append(o)

        if DBG == 1:
            return
        prologue(0)
        if DBG in (2, 21, 22, 23, 24, 25):
            return
        for t in range(NT):
            for c in range(NFC):
                gate_up(t, c)
                if c == 10 and t + 1 < NT:
                    prologue(t + 1)
            if DBG == 3:
                return
            for sub in range(4):
                down(t, sub)

    w_loaded = None

    def ffn_weights(self, l, which):
        P = self.P
        wgd = self.prm[which + "_w_gate"][l].rearrange("(kc p) f -> p kc f", p=128)
        wud = self.prm[which + "_w_up"][l].rearrange("(kc p) f -> p kc f", p=128)
        wdd = self.prm[which + "_w_down"][l].rearrange("(fc p) d -> p fc d", p=128)
        amb, P.ambient = P.ambient, []
        groups = [(i * 512, min(FF, (i + 1) * 512)) for i in range((FF + 511) // 512)]
        for gi, (a, b) in enumerate(groups):
            P.dma("pool", self.wg[:, :, a:b], wgd[:, :, a:b], writes=["wg%d" % c for c in range(a // 128, b // 128)])
            P.dma("pool", self.wu[:, :, a:b], wud[:, :, a:b], writes=["wu%d" % c for c in range(a // 128, b // 128)])
        for c0 in range(0, NFC, 2):
            P.dma("pool", self.wd[:, c0:c0 + 2, :], wdd[:, c0:c0 + 2, :], writes=["wd%d" % c0, "wd%d" % (c0 + 1)])
        P.ambient = amb
        self.w_loaded = (l, which)

    def barrier(self):
        self.P.op("pool", lambda e: e.memset(self.stat[:, 62:63], 0.0), writes=["regB"])

    def attn_phase(self, l, src, dst):
        if DBG == 41:
            return
        self.proj_phase(l, src)
        if DBG == 31 or DBG >= 41:
            return
        if self.stages > 3 * l + 2:
            self.ffn_weights(l, "ffn2")
        self.attn_core(l, moba=True)
        if DBG == 32:
            return
        self.attn_core(l, moba=False)
        self.outproj_phase(l, src, dst)

    def prologue_x(self, t, src, gpre, xst, hst, hTt, junk, htag):
        P, bank, stat = self.P, self.bank, self.stat
        for sub in range(4):
            r0 = t * TT + sub * 128
            xb, hb = xst[sub % 2], hst[sub % 2]
            xr, hr = "xst%d" % (sub % 2), "hst%d" % (sub % 2)
            P.dma("sp", xb[:], src[r0:r0 + 128, :], reads=["xs_r%d" % (r0 // 128)], writes=[xr])
            sc = (t * 4 + sub) % 8
            ss = stat[:, sc:sc + 1]
            P.op("act", lambda e, xb=xb, ss=ss: e.activation(out=junk[:], in_=xb[:], func=AF.Square, accum_out=ss),
                 reads=[xr], writes=["junk", "ss%d" % sc])
            P.op("act", lambda e, ss=ss: e.activation(out=ss, in_=ss, func=AF.Sqrt, scale=1.0 / D, bias=self.eps),
                 reads=["eps", "ss%d" % sc], writes=["ss%d" % sc])
            P.op("dve", lambda e, ss=ss: e.reciprocal(out=ss, in_=ss), reads=["ss%d" % sc], writes=["ss%d" % sc])
            P.op("dve", lambda e, xb=xb, hb=hb, ss=ss: e.scalar_tensor_tensor(
                out=hb[:], in0=xb[:], scalar=ss, in1=gpre[:], op0=ALU.mult, op1=ALU.mult),
                reads=[xr, "ss%d" % sc, "gpre"], writes=[hr])
            for c in range(8):
                bk = 4 + c // 2
                pv = bank[bk][:, :].bitcast(BF16)
                col = (c % 2) * 512 + sub * 128
                P.op("pe", lambda e, pv=pv, col=col, hb=hb, c=c: e.transpose(
                    out=pv[:, col:col + 128], in_=hb[:, c * 128:(c + 1) * 128], identity=self.ident[:]),
                    reads=[hr, "ident"], writes=["bank%d" % bk])
        for c in range(8):
            bk = 4 + c // 2
            pv = bank[bk][:, :].bitcast(BF16)
            a = (c % 2) * 512
            if bk % 2 == 0:
                P.op("act", lambda e, pv=pv, c=c, a=a: e.activation(out=hTt[:, c, :], in_=pv[:, a:a + 512], func=AF.Copy),
                     reads=["bank%d" % bk], writes=["%s_%d" % (htag, c)])
            else:
                P.op("dve", lambda e, pv=pv, c=c, a=a: e.tensor_copy(out=hTt[:, c, :], in_=pv[:, a:a + 512]),
                     reads=["bank%d" % bk], writes=["%s_%d" % (htag, c)])

    def epilogue_y(self, b0, r0, idx, src, dst, gpost, xst, tst, junk):
        P, bank, stat = self.P, self.bank, self.stat
        sc = 8 + idx % 8
        ss2 = [stat[:, sc + 8 * hf:sc + 8 * hf + 1] for hf in range(2)]
        ss = stat[:, sc:sc + 1]
        for hf in range(2):
            P.op("act", lambda e, hf=hf: e.activation(out=junk[:, hf * 512:(hf + 1) * 512], in_=bank[b0 + hf][:, :],
                                                    func=AF.Square, accum_out=ss2[hf]),
                 reads=["bank%d" % (b0 + hf)], writes=["junk", "ssy%d_%d" % (sc, hf)])
        P.op("dve", lambda e: e.tensor_tensor(out=ss, in0=ss2[0], in1=ss2[1], op=ALU.add),
             reads=["ssy%d_0" % sc, "ssy%d_1" % sc], writes=["ssy%d_0" % sc])
        P.op("act", lambda e: e.activation(out=ss, in_=ss, func=AF.Sqrt, scale=1.0 / D, bias=self.eps),
             reads=["eps", "ssy%d_0" % sc], writes=["ssy%d_0" % sc])
        P.op("dve", lambda e: e.reciprocal(out=ss, in_=ss), reads=["ssy%d_0" % sc], writes=["ssy%d_0" % sc])
        tb, xb = tst[idx % 2], xst[idx % 2]
        tr, xr = "tst%d" % (idx % 2), "xst%d" % (idx % 2)
        P.dma("sp", xb[:], src[r0:r0 + 128, :], reads=["xs_r%d" % (r0 // 128)], writes=[xr])
        for hf in range(2):
            P.op("dve", lambda e, hf=hf: e.scalar_tensor_tensor(
                out=tb[:, hf * 512:(hf + 1) * 512], in0=bank[b0 + hf][:, :], scalar=ss,
                in1=gpost[:, hf * 512:(hf + 1) * 512], op0=ALU.mult, op1=ALU.mult),
                reads=["bank%d" % (b0 + hf), "ssy%d_0" % sc, "gpost"], writes=[tr])
        P.op("dve", lambda e: e.tensor_tensor(out=tb[:], in0=tb[:], in1=xb[:], op=ALU.add), reads=[tr, xr], writes=[tr])
        o = P.dma("sp", dst[r0:r0 + 128, :], tb[:], reads=[tr], writes=["xs_r%d" % (r0 // 128)])
        if dst is self.out:
            self.out_dmas.append(o)

    def attn_consts(self):
        P = self.P
        NB = S // 256
        self.tri = self.sb("tri", [128, 128], BF16)
        self.btab = self.sb("btab", [128, 12, 32], F32)
        self.regB = self.sb_off
        tmpf = self.sb("tmpf", [128, 128], F32)
        t0 = self.sb("t0", [128, 32], F32)
        poshi = self.sb("poshi", [1, S], BF16)
        poslo = self.sb("poslo", [1, S], BF16)
        onehot = self.sb("onehot", [16, S], BF16)
        slr = [self.sb("slr", [2, S], BF16) for _ in range(4)]
        P.op("pool", lambda e: e.memset(tmpf[:], NEG), writes=["tmpf"])
        P.op("pool", lambda e: e.affine_select(out=tmpf[:], in_=tmpf[:], pattern=[[-1, 128]], compare_op=ALU.is_gt,
                                               fill=0.0, base=0, channel_multiplier=1), reads=["tmpf"], writes=["tmpf"])
        P.op("pool", lambda e: e.tensor_copy(out=self.tri[:], in_=tmpf[:]), reads=["tmpf"], writes=["tri"])
        P.op("pool", lambda e: e.iota(t0[:], pattern=[[-128, 32]], base=384, channel_multiplier=1,
                                      allow_small_or_imprecise_dtypes=True), writes=["t0"])
        self.slopes = [2.0 ** (-(i + 1)) for i in range(MH)] + [2.0 ** (-2 * (i + 1)) for i in range(DH)]
        for hh in range(12):
            P.op("pool", lambda e, hh=hh: e.tensor_scalar(out=self.btab[:, hh, :], in0=t0[:], scalar1=self.slopes[hh],
                                                         scalar2=None, op0=ALU.mult), reads=["t0"], writes=["btab"])
        P.op("pool", lambda e: e.iota(poshi[:], pattern=[[0, S // 512], [256, 2], [0, 256]], base=0, channel_multiplier=0,
                                      allow_small_or_imprecise_dtypes=True), writes=["poshi"])
        P.op("pool", lambda e: e.iota(poslo[:], pattern=[[0, S // 256], [1, 256]], base=0, channel_multiplier=0,
                                      allow_small_or_imprecise_dtypes=True), writes=["poslo"])
        P.op("pool", lambda e: e.memset(onehot[:], 1.0), writes=["onehot"])
        P.op("pool", lambda e: e.affine_select(out=onehot[:], in_=onehot[:], pattern=[[1, NB], [0, 256]], compare_op=ALU.is_equal,
                                               fill=0.0, base=0, channel_multiplier=-1), reads=["onehot"], writes=["onehot"])
        for h in range(MH):
            P.dma("sp", self.QA[h, 80:81, :], poshi[:], reads=["poshi"])
            P.dma("sp", self.QA[h, 81:82, :], poslo[:], reads=["poslo"])
            P.dma("sp", self.KA[h, 64:80, :], onehot[:], reads=["onehot"])
        for h in range(DH):
            for m in range(2):
                P.dma("sp", self.QD[h, m, 64:65, :], poshi[:], reads=["poshi"])
                P.dma("sp", self.QD[h, m, 65:66, :], poslo[:], reads=["poslo"])
        for hh in range(12):
            sl = slr[hh % 4]
            P.op("pool", lambda e, sl=sl, hh=hh: e.memset(sl[:], -8.0 * self.slopes[hh]), writes=["slr%d" % (hh % 4)])
            if hh < MH:
                P.dma("sp", self.KA[hh, 80:82, :], sl[:], reads=["slr%d" % (hh % 4)])
            else:
                for m in range(2):
                    P.dma("sp", self.KD[hh - MH, m, 64:66, :], sl[:], reads=["slr%d" % (hh % 4)])

    def proj_phase(self, l, src):
        P, bank, stat = self.P, self.bank, self.stat
        NB = S // 256
        self.barrier()
        self.sb_off = self.regB
        gpre = self.sb("gpre", [128, D], F32)
        xst = [self.sb("xst", [128, D], F32) for _ in range(2)]
        hst = [self.sb("hst", [128, D], BF16) for _ in range(2)]
        hT = [self.sb("hT", [128, 8, TT], BF16) for _ in range(2)]
        junk = self.sb("junk", [128, D], BF16)
        qst = [self.sb("qst", [128, TT], BF16) for _ in range(3)]
        vms = [self.sb("vms", [128, MH, 65], BF16) for _ in range(2)]
        vds = [self.sb("vds", [128, DH, 129], BF16) for _ in range(2)]
        kmf = self.sb("kmf", [128, 4, 16], F32)
        kmb = self.sb("kmb", [128, 4, 32], BF16)
        maskc = self.sb("maskc", [128, 16, 128], F32)
        mt1 = self.sb("mt1", [128, 16, 128], F32)
        gsb = [self.sb("gsb", [128, 128], F32) for _ in range(2)]
        mx8 = [self.sb("mx8", [128, MH, 8], F32) for _ in range(2)]
        selb = [self.sb("selb", [128, 128], BF16) for _ in range(2)]
        selT = [self.sb("selT", [128, TT], BF16) for _ in range(2)]
        win = self.nc.alloc_sbuf_tensor_at("win_%d" % l, [128, 8, INW], BF16, offset=SB_BASE)
        wind = self.prm["w_in"][l].rearrange("(kc p) f -> p kc f", p=128)
        allw = ["wg%d" % c for c in range(NFC)] + ["wu%d" % c for c in range(NFC)]
        for g in range(6):
            P.dma("pool", win[:, :, g * 512:(g + 1) * 512], wind[:, :, g * 512:(g + 1) * 512],
                  writes=(allw if g == 0 else []) + ["win%d" % g])
        P.dma("pool", gpre[:], self.prm["mix_pre_g"][l].partition_broadcast(128), writes=["gpre"])
        P.op("pool", lambda e: e.memset(kmf[:], 0.0), writes=["kmf"])
        P.op("pool", lambda e: e.memset(kmb[:], 0.0), writes=["kmb"])
        for b_ in range(2):
            P.op("pool", lambda e, b_=b_: e.memset(vms[b_][:, :, 64:65], 1.0), writes=["vms%d" % b_])
            P.op("pool", lambda e, b_=b_: e.memset(vds[b_][:, :, 128:129], 1.0), writes=["vds%d" % b_])
        P.op("pool", lambda e: e.iota(mt1[:], pattern=[[-1, 16], [0, MH], [1, 16]], base=0, channel_multiplier=0,
                                      allow_small_or_imprecise_dtypes=True), writes=["mt1"])
        P.op("pool", lambda e: e.tensor_scalar(out=maskc[:], in0=mt1[:], scalar1=0.0, scalar2=-1e30, op0=ALU.is_gt, op1=ALU.mult),
             reads=["mt1"], writes=["maskc"])
        P.op("pool", lambda e: e.tensor_scalar(out=mt1[:], in0=mt1[:], scalar1=0.0, scalar2=1e30, op0=ALU.is_equal, op1=ALU.mult),
             reads=["mt1", "maskc"], writes=["mt1"])
        P.op("pool", lambda e: e.tensor_tensor(out=maskc[:], in0=maskc[:], in1=mt1[:], op=ALU.add),
             reads=["mt1", "maskc"], writes=["maskc"])
        wr = ["wg0"]
        cnt = {"pb": 0, "q": 0}
        if DBG == 42:
            return

        def fm_chunk(t, oc):
            bk = cnt["pb"] % 2
            cnt["pb"] += 1
            for kc in range(8):
                P.op("pe", lambda e, kc=kc, bk=bk: e.matmul(bank[bk][:, :], lhsT=win[:, kc, oc * 128:(oc + 1) * 128],
                                                           rhs=hT[t % 2][:, kc, :], start=(kc == 0), stop=(kc == 7)),
                     reads=["win%d" % (oc // 4), "hTp%d_%d" % (t % 2, kc)] + wr, writes=["bank%d" % bk])
            return bk

        def evac_q(bk):
            qi = cnt["q"] % 3
            cnt["q"] += 1
            P.op("act", lambda e: e.activation(out=qst[qi][:], in_=bank[bk][:, :], func=AF.Copy),
                 reads=["bank%d" % bk], writes=["qst%d" % qi])
            return qi

        cols = lambda t: slice(t * TT, (t + 1) * TT)
        self.prologue_x(0, src, gpre, xst, hst, hT[0], junk, "hTp0")
        for t in range(NT):
            for oc in range(4, 8):
                bk = fm_chunk(t, oc)
                qi = evac_q(bk)
                P.op("dve", lambda e, bk=bk, oc=oc, t=t: e.tensor_reduce(
                    out=kmf[:, oc - 4, 2 * t:2 * t + 2], in_=bank[bk][:, :].rearrange("p (a b) -> p a b", a=2),
                    axis=AX.X, op=ALU.add), reads=["bank%d" % bk, "kmf"], writes=["kmf"])
                for hh in range(2):
                    P.dma("sp", self.KA[2 * (oc - 4) + hh, 0:64, cols(t)], qst[qi][hh * 64:(hh + 1) * 64, :],
                          reads=["qst%d" % qi], writes=["KA"])
            for hh in range(2):
                P.op("dve", lambda e, hh=hh: e.tensor_scalar(
                    out=kmb[hh * 64:(hh + 1) * 64, :, hh * 16:(hh + 1) * 16], in0=kmf[hh * 64:(hh + 1) * 64, :, :],
                    scalar1=1.0 / 256, scalar2=None, op0=ALU.mult), reads=["kmf"], writes=["kmb"])
            if t + 1 < NT:
                self.prologue_x(t + 1, src, gpre, xst, hst, hT[(t + 1) % 2], junk, "hTp%d" % ((t + 1) % 2))
            for oc in range(0, 4):
                bk = fm_chunk(t, oc)
                qi = evac_q(bk)
                for hh in range(2):
                    P.dma("sp", self.QA[2 * oc + hh, 0:64, cols(t)], qst[qi][hh * 64:(hh + 1) * 64, :],
                          reads=["qst%d" % qi], writes=["QA"])
                for s_ in range(4):
                    P.op("pe", lambda e, s_=s_, qi=qi, oc=oc: e.matmul(
                        bank[3][:, s_ * 128 + oc * 32:s_ * 128 + oc * 32 + 32],
                        lhsT=qst[qi][:, s_ * 128:(s_ + 1) * 128],
                        rhs=kmb[:, oc, :], start=True, stop=True, skip_group_check=True),
                        reads=["qst%d" % qi, "kmb"], writes=["bank3"])
            pvT = bank[2][:, :].bitcast(BF16)
            for s_ in range(4):
                cur = 2 * t + s_ // 2
                gb, mb, sbb = gsb[s_ % 2], mx8[s_ % 2], selb[s_ % 2]
                gr, mr, sr = "gsb%d" % (s_ % 2), "mx8%d" % (s_ % 2), "selb%d" % (s_ % 2)
                P.op("dve", lambda e, s_=s_, cur=cur, gb=gb: e.tensor_tensor(
                    out=gb[:], in0=bank[3][:, s_ * 128:(s_ + 1) * 128], in1=maskc[:, cur, :], op=ALU.add),
                    reads=["bank3", "maskc"], writes=[gr])
                for h in range(MH):
                    P.op("dve", lambda e, h=h, gb=gb, mb=mb: e.max(out=mb[:, h, :], in_=gb[:, h * 16:(h + 1) * 16]),
                         reads=[gr], writes=[mr])
                for h in range(MH):
                    P.op("dve", lambda e, h=h, gb=gb, mb=mb, sbb=sbb: e.tensor_scalar(
                        out=sbb[:, h * 16:(h + 1) * 16], in0=gb[:, h * 16:(h + 1) * 16], scalar1=mb[:, h, 3:4], scalar2=NEG,
                        op0=ALU.is_lt, op1=ALU.mult), reads=[gr, mr], writes=[sr])
                P.op("pe", lambda e, s_=s_, sbb=sbb: e.transpose(out=pvT[:, s_ * 128:(s_ + 1) * 128], in_=sbb[:], identity=self.ident[:]),
                     reads=[sr, "ident"], writes=["bank2"])
            sT = selT[t % 2]
            P.op("act", lambda e, sT=sT: e.activation(out=sT[:], in_=pvT[:, 0:512], func=AF.Copy),
                 reads=["bank2"], writes=["selT%d" % (t % 2)])
            for h in range(MH):
                P.dma("sp", self.QA[h, 64:80, cols(t)], sT[h * 16:(h + 1) * 16, :], reads=["selT%d" % (t % 2)], writes=["QA"])
            if DBG == 44:
                continue
            for oc in range(12, 20):
                bk = fm_chunk(t, oc)
                qi = evac_q(bk)
                dstT = self.QD if oc < 16 else self.KD
                hd = (oc - 12) % 4
                for m in range(2):
                    P.dma("sp", dstT[hd, m, 0:64, cols(t)], qst[qi][m * 64:(m + 1) * 64, :],
                          reads=["qst%d" % qi], writes=["QD"])
            if DBG == 45:
                continue
            for s_ in range(4):
                r0 = t * TT + s_ * 128
                for which in range(2):
                    bk = cnt["pb"] % 2
                    cnt["pb"] += 1
                    c0 = 1024 if which == 0 else 2560
                    for kc in range(8):
                        P.op("pe", lambda e, kc=kc, bk=bk, c0=c0, s_=s_, t=t: e.matmul(
                            bank[bk][:, :], lhsT=hT[t % 2][:, kc, s_ * 128:(s_ + 1) * 128], rhs=win[:, kc, c0:c0 + 512],
                            start=(kc == 0), stop=(kc == 7)),
                            reads=["win%d" % (c0 // 512), "hTp%d_%d" % (t % 2, kc)] + wr, writes=["bank%d" % bk])
                    if which == 0:
                        vb = vms[s_ % 2]
                        P.op("act", lambda e, bk=bk, vb=vb: e.activation(
                            out=vb[:, :, 0:64], in_=bank[bk][:, :].rearrange("p (h d) -> p h d", h=MH), func=AF.Copy),
                            reads=["bank%d" % bk], writes=["vms%d" % (s_ % 2)])
                        P.dma("sp", self.VM[r0:r0 + 128, :], vb[:].rearrange("p h d -> p (h d)"),
                              reads=["vms%d" % (s_ % 2)], writes=["VM"])
                    else:
                        vb = vds[s_ % 2]
                        P.op("dve", lambda e, bk=bk, vb=vb: e.tensor_copy(
                            out=vb[:, :, 0:128], in_=bank[bk][:, :].rearrange("p (h d) -> p h d", h=DH)),
                            reads=["bank%d" % bk], writes=["vds%d" % (s_ % 2)])
                        P.dma("sp", self.VD[r0:r0 + 128, :], vb[:].rearrange("p h d -> p (h d)"),
                              reads=["vds%d" % (s_ % 2)], writes=["VD"])

    def attn_core(self, l, moba):
        P, bank, stat = self.P, self.bank, self.stat
        NKT = S // 128
        self.barrier()
        self.sb_off = self.regB
        nh = MH if moba else DH
        nm = 1 if moba else 2
        K = 82 if moba else 66
        dv = 64 if moba else 128
        vw = nh * (dv + 1)
        Vs = self.sb("Vs", [128, NKT, vw], BF16)
        Vd = (self.VM if moba else self.VD).rearrange("(i p) c -> p i c", p=128)
        step = max(1, NKT // 4)
        for i0 in range(0, NKT, step):
            P.dma("sp", Vs[:, i0:i0 + step, :], Vd[:, i0:i0 + step, :], reads=["VM", "VD"], writes=["Vs"])
        nqb = 2 if moba else 1
        Qb = [[self.sb("Qb", [128, S], BF16) for m in range(nm)] for _ in range(nqb)]
        Kb = [[self.sb("Kb", [128, S], BF16) for m in range(nm)] for _ in range(nqb)]
        for a_ in range(nqb):
            for m in range(nm):
                P.op("pool", lambda e, a_=a_, m=m: e.memset(Qb[a_][m][:], 0.0), writes=["Qb%d_%d" % (a_, m), "QKz"])
                P.op("pool", lambda e, a_=a_, m=m: e.memset(Kb[a_][m][:], 0.0), writes=["Kb%d_%d" % (a_, m), "QKz"])
        npt = 4 if moba else 3
        pt = [self.sb("pt", [128, 512], BF16) for _ in range(npt)]
        ostg = [self.sb("ostg", [128, 4, dv], BF16) for _ in range(2)]
        rz = self.sb("rz", [128, 16], F32)
        if not moba:
            lams = self.sb("lams", [128, 8], F32)
            subg = self.sb("subg", [128, 128], F32)
            t1off = self.sb_off
            t1 = [self.sb("t1", [128, 128], F32) for _ in range(2)]
            lamt = self.sb("lamt", [128, 4, 64], F32, off=t1off)
            asb = [self.sb("asb", [128, 128], F32) for _ in range(2)]
            junk = self.sb("junk", [128, 128], BF16)
            lam_init = 0.8 - 0.6 * math.exp(-0.3 * l)
            for i, nm_ in enumerate(["lambda_q1", "lambda_k1", "lambda_q2", "lambda_k2"]):
                P.dma("sp", lamt[:, i, :], self.prm[nm_][l].partition_broadcast(128), writes=["lamt"])
            P.dma("sp", subg[:], self.prm["subln_g"][l].partition_broadcast(128), writes=["subg"])
            P.op("dve", lambda e: e.tensor_scalar(out=subg[:], in0=subg[:], scalar1=1.0 - lam_init, scalar2=None, op0=ALU.mult),
                 reads=["subg"], writes=["subg"])
            for i in range(2):
                P.op("dve", lambda e, i=i: e.tensor_tensor(out=lamt[:, 2 * i, :], in0=lamt[:, 2 * i, :], in1=lamt[:, 2 * i + 1, :], op=ALU.mult),
                     reads=["lamt"], writes=["lamt"])
                P.op("dve", lambda e, i=i: e.tensor_reduce(out=lams[:, i:i + 1], in_=lamt[:, 2 * i, :], axis=AX.X, op=ALU.add),
                     reads=["lamt"], writes=["lams"])
                P.op("act", lambda e, i=i: e.activation(out=lams[:, i:i + 1], in_=lams[:, i:i + 1], func=AF.Exp),
                     reads=["lams"], writes=["lams"])
            P.op("dve", lambda e: e.tensor_tensor(out=lams[:, 2:3], in0=lams[:, 1:2], in1=lams[:, 0:1], op=ALU.subtract),
                 reads=["lams"], writes=["lams"])
            P.op("dve", lambda e: e.tensor_scalar(out=lams[:, 2:3], in0=lams[:, 2:3], scalar1=-lam_init, scalar2=None, op0=ALU.add),
                 reads=["lams"], writes=["lams"])
            neglam = lams[:, 2:3]
        sbanks = [0, 1, 2] if moba else [0, 1]
        osets = [[4], [5], [6], [7]] if moba else [[2, 3, 4], [5, 6, 7]]
        NQ = S // 512
        units = []
        for h in range(nh):
            for j in range(NQ):
                oset = osets[(h * NQ + j) % len(osets)]
                for i in range(4 * j + 4):
                    for m in range(nm):
                        units.append(dict(h=h, j=j, i=i, m=m, oset=oset, first=(j == 0 and i == 0 and m == 0),
                                          last=(i == 4 * j + 3 and m == nm - 1)))
        started = {}

        def acc(u, s_, m):
            if moba:
                return u["oset"][0], s_ * 65
            a = s_ * 2 + m
            return u["oset"][a // 3], (a % 3) * 129

        def load_head(h):
            qb = h % nqb
            for m in range(nm):
                qsrc = self.QA[h] if moba else self.QD[h, m]
                ksrc = self.KA[h] if moba else self.KD[h, m]
                P.dma("sp", Qb[qb][m][0:K, :], qsrc, reads=["QA", "QD"], writes=["Qb%d_%d" % (qb, m)])
                P.dma("sp", Kb[qb][m][0:K, :], ksrc, reads=["KA", "KD"], writes=["Kb%d_%d" % (qb, m)])

        def stageA(n):
            u = units[n]
            h, j, i, m = u["h"], u["j"], u["i"], u["m"]
            if u["first"]:
                if h == 0 or nqb == 1:
                    load_head(h)
                if nqb == 2 and h + 1 < nh:
                    load_head(h + 1)
            qb = h % nqb
            hh = h if moba else MH + h
            d = i - 4 * j
            qlo = 128 * d if d > 0 else 0
            sb_ = sbanks[n % len(sbanks)]
            pb = n % npt
            P.op("pe", lambda e: e.matmul(
                bank[sb_][:, qlo:512], lhsT=Kb[qb][m][0:K, i * 128:(i + 1) * 128],
                rhs=Qb[qb][m][0:K, j * 512 + qlo:(j + 1) * 512], start=True, stop=(d < 0)),
                reads=["Kb%d_%d" % (qb, m), "Qb%d_%d" % (qb, m), "QKz"], writes=["bank%d" % sb_])
            if d >= 0:
                P.op("pe", lambda e: e.matmul(
                    bank[sb_][:, qlo:qlo + 128], lhsT=self.ident[:], rhs=self.tri[:], start=False, stop=True),
                    reads=["ident", "tri"], writes=["bank%d" % sb_])
            P.op("act", lambda e: e.activation(
                out=pt[pb][:, qlo:512], in_=bank[sb_][:, qlo:512], func=AF.Exp, scale=0.125,
                bias=self.btab[:, hh, 4 * j - i + 3:4 * j - i + 4]),
                reads=["bank%d" % sb_, "btab"], writes=["pt%d" % pb])

        def stageB(n):
            u = units[n]
            h, j, i, m = u["h"], u["j"], u["i"], u["m"]
            d = i - 4 * j
            pb = n % npt
            key = (h, j)
            st_set = started.setdefault(key, set())
            for s_ in range(max(d, 0), 4):
                bk, c0 = acc(u, s_, m)
                st = bk not in st_set
                st_set.add(bk)
                P.op("pe", lambda e, bk=bk, c0=c0, s_=s_, st=st: e.matmul(
                    bank[bk][:, c0:c0 + dv + 1], lhsT=pt[pb][:, s_ * 128:(s_ + 1) * 128],
                    rhs=Vs[:, i, h * (dv + 1):(h + 1) * (dv + 1)], start=st, stop=(i == 4 * j + s_),
                    skip_group_check=True),
                    reads=["pt%d" % pb, "Vs"], writes=["bank%d" % bk])
            if u["last"]:
                epilogue(u)

        ecount = {"n": 0}

        def epilogue(u):
            h, j = u["h"], u["j"]
            en = ecount["n"]
            ecount["n"] += 1
            og = ostg[en % 2]
            orr = "ostg%d" % (en % 2)
            if moba:
                bk = u["oset"][0]
                ov = bank[bk][:, 0:260].rearrange("p (s c) -> p s c", s=4)
                P.op("dve", lambda e: e.reciprocal(out=rz[:, 0:4], in_=ov[:, :, 64]), reads=["bank%d" % bk], writes=["rz"])
                for s_ in range(4):
                    P.op("dve", lambda e, s_=s_: e.tensor_scalar(
                        out=og[:, s_, :], in0=ov[:, s_, 0:64], scalar1=rz[:, s_:s_ + 1], scalar2=None, op0=ALU.mult),
                        reads=["bank%d" % bk, "rz"], writes=[orr])
                P.dma("sp", self.MIX[j * 512:(j + 1) * 512, h * 64:(h + 1) * 64].rearrange("(s p) c -> p s c", p=128),
                      og[:], reads=[orr], writes=["MIX"])
                return
            for s_ in range(4):
                b1, c1 = acc(u, s_, 0)
                b2, c2 = acc(u, s_, 1)
                tt, aa = t1[s_ % 2], asb[s_ % 2]
                tr, ar = "t1%d" % (s_ % 2), "asb%d" % (s_ % 2)
                r1, r2, ssq = rz[:, 4 * s_:4 * s_ + 1], rz[:, 4 * s_ + 1:4 * s_ + 2], rz[:, 4 * s_ + 2:4 * s_ + 3]
                rr = "rz%d" % s_
                P.op("dve", lambda e, b1=b1, c1=c1, r1=r1: e.reciprocal(out=r1, in_=bank[b1][:, c1 + 128:c1 + 129]),
                     reads=["bank%d" % b1], writes=[rr])
                P.op("dve", lambda e, b2=b2, c2=c2, r2=r2: e.reciprocal(out=r2, in_=bank[b2][:, c2 + 128:c2 + 129]),
                     reads=["bank%d" % b2, rr], writes=[rr])
                P.op("dve", lambda e, r2=r2: e.tensor_tensor(out=r2, in0=r2, in1=neglam, op=ALU.mult),
                     reads=[rr, "lams"], writes=[rr])
                P.op("dve", lambda e, b1=b1, c1=c1, r1=r1, tt=tt: e.tensor_scalar(
                    out=tt[:], in0=bank[b1][:, c1:c1 + 128], scalar1=r1, scalar2=None, op0=ALU.mult),
                    reads=["bank%d" % b1, rr], writes=[tr])
                P.op("dve", lambda e, b2=b2, c2=c2, r2=r2, tt=tt, aa=aa: e.scalar_tensor_tensor(
                    out=aa[:], in0=bank[b2][:, c2:c2 + 128], scalar=r2, in1=tt[:], op0=ALU.mult, op1=ALU.add),
                    reads=["bank%d" % b2, rr, tr], writes=[ar])
                P.op("act", lambda e, aa=aa, ssq=ssq: e.activation(out=junk[:], in_=aa[:], func=AF.Square, accum_out=ssq),
                     reads=[ar, rr], writes=["junkd", rr])
                P.op("act", lambda e, ssq=ssq: e.activation(out=ssq, in_=ssq, func=AF.Sqrt, scale=1.0 / 128, bias=self.eps),
                     reads=["eps", rr], writes=[rr])
                P.op("dve", lambda e, ssq=ssq: e.reciprocal(out=ssq, in_=ssq), reads=[rr], writes=[rr])
                P.op("dve", lambda e, aa=aa, ssq=ssq, s_=s_: e.scalar_tensor_tensor(
                    out=og[:, s_, :], in0=aa[:], scalar=ssq, in1=subg[:], op0=ALU.mult, op1=ALU.mult),
                    reads=[ar, rr, "subg"], writes=[orr])
            P.dma("sp", self.MIX[j * 512:(j + 1) * 512, 512 + h * 128:512 + (h + 1) * 128].rearrange("(s p) c -> p s c", p=128),
                  og[:], reads=[orr], writes=["MIX"])

        stageA(0)
        for n in range(len(units)):
            if n + 1 < len(units):
                stageA(n + 1)
            stageB(n)


    def outproj_phase(self, l, src, dst):
        P, bank, stat = self.P, self.bank, self.stat
        self.barrier()
        self.sb_off = self.regB
        wo = self.sb("wo", [128, 8, D], BF16)
        gpost = self.sb("gpost", [128, D], F32)
        mst = [self.sb("mst", [128, D], BF16) for _ in range(2)]
        mT = [self.sb("mT", [128, 8, 128], BF16) for _ in range(2)]
        xst = [self.sb("xst", [128, D], F32) for _ in range(2)]
        tst = [self.sb("tst", [128, D], F32) for _ in range(2)]
        junk = self.sb("junk", [128, D], BF16)
        wod = self.prm["w_out"][l].rearrange("(kc p) f -> p kc f", p=128)
        for g in range(2):
            P.dma("pool", wo[:, :, g * 512:(g + 1) * 512], wod[:, :, g * 512:(g + 1) * 512], writes=["wo%d" % g])
        P.dma("pool", gpost[:], self.prm["mix_post_g"][l].partition_broadcast(128), writes=["gpost"])
        for r in range(S // 128):
            r0 = r * 128
            mb, mtb = mst[r % 2], mT[r % 2]
            P.dma("sp", mb[:], self.MIX[r0:r0 + 128, :], reads=["MIX"], writes=["mst%d" % (r % 2)])
            tb_ = r % 2
            pv = bank[tb_][:, :].bitcast(BF16)
            for c in range(8):
                P.op("pe", lambda e, pv=pv, c=c, mb=mb: e.transpose(out=pv[:, c * 128:(c + 1) * 128], in_=mb[:, c * 128:(c + 1) * 128],
                                                                  identity=self.ident[:]),
                     reads=["mst%d" % (r % 2), "ident"], writes=["bank%d" % tb_])
            if tb_ == 0:
                P.op("act", lambda e, pv=pv, mtb=mtb: e.activation(out=mtb[:].rearrange("p c t -> p (c t)"), in_=pv[:, :], func=AF.Copy),
                     reads=["bank%d" % tb_], writes=["mT%d" % (r % 2)])
            else:
                P.op("dve", lambda e, pv=pv, mtb=mtb: e.tensor_copy(out=mtb[:].rearrange("p c t -> p (c t)"), in_=pv[:, :]),
                     reads=["bank%d" % tb_], writes=["mT%d" % (r % 2)])
            b0 = 4 + 2 * (r % 2)
            for hf in range(2):
                for c in range(8):
                    P.op("pe", lambda e, c=c, hf=hf, mtb=mtb, b0=b0: e.matmul(bank[b0 + hf][:, :], lhsT=mtb[:, c, :],
                                                                     rhs=wo[:, c, hf * 512:(hf + 1) * 512], start=(c == 0), stop=(c == 7)),
                         reads=["mT%d" % (r % 2), "wo%d" % hf], writes=["bank%d" % (b0 + hf)])
            self.epilogue_y(b0, r0, r, src, dst, gpost, xst, tst, junk)


_NC_CACHE = {}


def get_nc(stages):
    if stages not in _NC_CACHE:
        _NC_CACHE[stages] = Builder(stages).build()
    return _NC_CACHE[stages]


STAGES = 6
DBG = 0


def kernel(**inputs):
    nc = get_nc(STAGES)
    x = np.ascontiguousarray(inputs["x"], dtype=np.float32)
    shared = {k: np.ascontiguousarray(v, dtype=np.float32) for k, v in inputs.items() if k != "x"}
    in_maps = []
    for b in range(8):
        m = dict(shared)
        m["x"] = x[b]
        in_maps.append(m)
    res = run_bass_kernel_spmd(nc, in_maps, core_ids=list(range(8)))
    return np.stack([res.results[b]["out"] for b in range(8)], axis=0)
```

```python
import math
import contextlib
import numpy as np
import concourse.bass as bass
import concourse.mybir as mybir
from concourse.bass_utils import run_bass_kernel_spmd

F32 = mybir.dt.float32
BF16 = mybir.dt.bfloat16
AF = mybir.ActivationFunctionType
ALU = mybir.AluOpType
AX = mybir.AxisListType

D = 1024
S = 4096
L = 2
FF = 2816
NFC = FF // 128
HD = 64
MH = 8
DH = 4
INW = 3072
EPS = 1e-6
TT = 512
NT = S // TT
NEG = -30000.0
NQST = 6


class Res:
    __slots__ = ("name", "last_write", "reads")

    def __init__(self, name):
        self.name = name
        self.last_write = None
        self.reads = []


class Op:
    __slots__ = ("eng", "fn", "seq", "waits", "signal", "sig", "is_dma", "dsem", "dval", "dprev")

    def __init__(self, eng, fn, is_dma=False):
        self.eng = eng
        self.fn = fn
        self.seq = 0
        self.waits = {}
        self.signal = False
        self.sig = 0
        self.is_dma = is_dma
        self.dsem = None
        self.dval = 0
        self.dprev = None


class Prog:
    ENGS = ("pe", "act", "dve", "pool", "sp")

    def __init__(self, nc, ndma_sems=12):
        self.nc = nc
        self.ops = {e: [] for e in self.ENGS}
        self.known = {e: {} for e in self.ENGS}
        self.dma_count = {e: 0 for e in self.ENGS}
        self.dma_ops = {e: [] for e in self.ENGS}
        self.ndma = ndma_sems
        self.res = {}
        self.ambient = []

    def R(self, name):
        r = self.res.get(name)
        if r is None:
            r = self.res[name] = Res(name)
        return r

    def _dep(self, a, b):
        if a is None or a is b:
            return
        if a.is_dma:
            key = ("dma",) + a.dsem
            if self.known[b.eng].get(key, 0) >= a.dval:
                return
            self.known[b.eng][key] = a.dval
            b.waits[key] = a
            return
        if a.eng == b.eng and a.eng == "pe":
            return
        k = self.known[b.eng].get(a.eng, 0)
        if a.seq <= k:
            return
        cur = b.waits.get(a.eng)
        if cur is None or cur.seq < a.seq:
            b.waits[a.eng] = a
        a.signal = True

    def op(self, eng, fn, reads=(), writes=(), is_dma=False):
        o = Op(eng, fn, is_dma)
        lst = self.ops[eng]
        lst.append(o)
        o.seq = len(lst)
        writes = list(writes) + [r for r in reads if isinstance(r, str) and r.startswith("bank")]
        reads = [r for r in reads if not (isinstance(r, str) and r.startswith("bank"))]
        reads = [self.R(r) if isinstance(r, str) else r for r in list(reads) + self.ambient]
        writes = [self.R(w) if isinstance(w, str) else w for w in writes]
        for r in reads:
            self._dep(r.last_write, o)
        for w in writes:
            self._dep(w.last_write, o)
            for rd in w.reads:
                self._dep(rd, o)
        for r in reads:
            r.reads.append(o)
        for w in writes:
            w.last_write = o
            w.reads = []
        for k, a in o.waits.items():
            if not isinstance(k, tuple):
                self.known[eng][k] = max(self.known[eng].get(k, 0), a.seq)
        if is_dma:
            i = self.dma_count[eng]
            self.dma_count[eng] = i + 1
            o.dsem = (eng, i % self.ndma)
            o.dval = 16 * (i // self.ndma + 1)
            if i >= self.ndma:
                o.dprev = self.dma_ops[eng][i - self.ndma]
            self.dma_ops[eng].append(o)
        return o

    def dma(self, eng, out, in_, reads=(), writes=(), **kw):
        return self.op(eng, lambda e: e.dma_start(out=out, in_=in_, **kw), reads, writes, is_dma=True)

    def emit(self, final_waits=()):
        nc = self.nc
        engobj = {"pe": "tensor", "act": "scalar", "dve": "vector", "pool": "gpsimd", "sp": "sync"}
        with contextlib.ExitStack() as st:
            sems = {e: st.enter_context(nc.semaphore("s_" + e)) for e in self.ENGS}
            dsems = {}
            for e in self.ENGS:
                for k in range(min(self.ndma, self.dma_count[e])):
                    dsems[(e, k)] = st.enter_context(nc.semaphore("d_%s_%d" % (e, k)))
            for e in self.ENGS:
                c = 0
                for o in self.ops[e]:
                    if o.signal and not o.is_dma:
                        c += 1
                        o.sig = c
            block = st.enter_context(nc.Block())

            def run(e):
                def body(eng):
                    for o in self.ops[e]:
                        if o.dprev is not None:
                            eng.wait_ge(dsems[o.dprev.dsem], o.dprev.dval)
                        for k, a in o.waits.items():
                            if a.is_dma:
                                eng.wait_ge(dsems[a.dsem], a.dval)
                            else:
                                eng.wait_ge(sems[a.eng], a.sig)
                        ins = o.fn(eng)
                        if o.is_dma:
                            ins.then_inc(dsems[o.dsem], 16)
                        elif o.signal:
                            ins.then_inc(sems[e], 1)
                    if e == "sp":
                        for a in final_waits:
                            eng.wait_ge(dsems[a.dsem], a.dval)
                return body

            for e in self.ENGS:
                getattr(block, engobj[e])(run(e))


SB_BASE = 16512
SB_END = 229376


class Builder:
    def __init__(self, stages):
        self.stages = stages
        nc = self.nc = bass.Bass("TRN2", target_bir_lowering=False)
        self.P = Prog(nc)
        self.sb_off = SB_BASE
        self.uid = 0
        dt = lambda name, shape, dtype=F32, kind="ExternalInput": nc.dram_tensor(name, shape, dtype, kind=kind).ap()
        self.x_in = dt("x", [S, D])
        self.prm = {}
        for name, shape in [
            ("ffn1_pre_g", [L, D]), ("ffn1_w_gate", [L, D, FF]), ("ffn1_w_up", [L, D, FF]), ("ffn1_w_down", [L, FF, D]),
            ("ffn1_post_g", [L, D]), ("mix_pre_g", [L, D]), ("w_in", [L, D, INW]),
            ("lambda_q1", [L, HD]), ("lambda_k1", [L, HD]), ("lambda_q2", [L, HD]), ("lambda_k2", [L, HD]),
            ("subln_g", [L, 128]), ("w_out", [L, D, D]), ("mix_post_g", [L, D]),
            ("ffn2_pre_g", [L, D]), ("ffn2_w_gate", [L, D, FF]), ("ffn2_w_up", [L, D, FF]), ("ffn2_w_down", [L, FF, D]),
            ("ffn2_post_g", [L, D]),
        ]:
            self.prm[name] = dt(name, shape)
        self.out = dt("out", [S, D], F32, "ExternalOutput")
        self.xs = dt("xs", [S, D], F32, "Internal")
        self.bank = [nc.alloc_psum_tensor("bank%d" % i, [128, 512], F32) for i in range(8)]
        self.out_dmas = []
        self.QA = dt("QA", [MH, 82, S], BF16, "Internal")
        self.KA = dt("KA", [MH, 82, S], BF16, "Internal")
        self.VM = dt("VM", [S, MH * 65], BF16, "Internal")
        self.QD = dt("QD", [DH, 2, 66, S], BF16, "Internal")
        self.KD = dt("KD", [DH, 2, 66, S], BF16, "Internal")
        self.VD = dt("VD", [S, DH * 129], BF16, "Internal")
        self.MIX = dt("MIX", [S, D], BF16, "Internal")

    def sb(self, name, shape, dtype, off=None):
        esz = 2 if dtype == BF16 else 4
        n = esz
        for s_ in shape[1:]:
            n *= s_
        n = (n + 31) // 32 * 32
        if off is None:
            off = self.sb_off
            self.sb_off += n
            assert self.sb_off <= SB_END, (name, self.sb_off)
        self.uid += 1
        return self.nc.alloc_sbuf_tensor_at("%s_%d" % (name, self.uid), shape, dtype, offset=off)

    def build(self):
        P = self.P
        nc = self.nc
        self.wg = self.sb("wg", [128, 8, FF], BF16)
        self.wu = self.sb("wu", [128, 8, FF], BF16)
        self.wd = self.sb("wd", [128, NFC, D], BF16)
        self.ident = self.sb("ident", [128, 128], BF16)
        self.identf = self.sb("identf", [128, 128], F32)
        self.stat = self.sb("stat", [128, 64], F32)
        self.regB = self.sb_off
        self.eps = self.stat[:, 63:64]
        P.op("pool", lambda e: e.memset(self.eps, EPS), writes=["eps"])
        P.ambient = ["regB"]
        onesf = self.identf
        P.op("pool", lambda e: e.memset(onesf[:], 1.0), writes=["identf"])
        P.op("pool", lambda e: e.affine_select(out=onesf[:], in_=onesf[:], pattern=[[-1, 128]], compare_op=ALU.is_equal,
                                               fill=0.0, base=0, channel_multiplier=1), reads=["identf"], writes=["identf"])
        P.op("pool", lambda e: e.tensor_copy(out=self.ident[:], in_=onesf[:]), reads=["identf"], writes=["ident"])
        if self.stages > 1:
            self.attn_consts()

        src = self.x_in
        nst = 0
        for l in range(L):
            for which in ("ffn1", "attn", "ffn2"):
                if nst >= self.stages:
                    break
                nst += 1
                last = (nst == self.stages) or (l == L - 1 and which == "ffn2")
                dst = self.out if last else self.xs
                if which == "attn":
                    self.attn_phase(l, src, dst)
                elif DBG < 41:
                    self.ffn_phase(l, which, src, dst)
                src = self.xs
        P.emit(final_waits=self.out_dmas)
        return nc

    def rows(self, t_ap, r0, n=128):
        return t_ap[r0:r0 + n, :]

    def ffn_phase(self, l, which, src, dst):
        P = self.P
        nc = self.nc
        is_out = dst is self.out
        pre_g = self.prm[which + "_pre_g"]
        post_g = self.prm[which + "_post_g"]
        wgd = self.prm[which + "_w_gate"][l].rearrange("(kc p) f -> p kc f", p=128)
        wud = self.prm[which + "_w_up"][l].rearrange("(kc p) f -> p kc f", p=128)
        wdd = self.prm[which + "_w_down"][l].rearrange("(fc p) d -> p fc d", p=128)
        self.barrier()
        self.sb_off = self.regB
        gpre = self.sb("gpre", [128, D], F32)
        gpost = self.sb("gpost", [128, D], F32)
        xst = [self.sb("xst", [128, D], F32) for _ in range(2)]
        hst = [self.sb("hst", [128, D], BF16) for _ in range(2)]
        hT = [self.sb("hT", [128, 8, TT], BF16) for _ in range(2)]
        sg = [self.sb("sg", [128, TT], BF16) for _ in range(2)]
        uT = self.sb("uT", [128, NFC, TT], BF16)
        junk = self.sb("junk", [128, D], BF16)
        tst = [self.sb("tst", [128, D], F32) for _ in range(2)]
        stat = self.stat
        tag = "%s%d" % (which, l)
        bank = self.bank

        P.dma("pool", gpre[:], pre_g[l].partition_broadcast(128), writes=["gpre"])
        P.dma("pool", gpost[:], post_g[l].partition_broadcast(128), writes=["gpost"])
        if self.w_loaded != (l, which):
            self.ffn_weights(l, which)
        P.op("pool", lambda e: e.tensor_scalar(out=gpost[:], in0=gpost[:], scalar1=0.5, scalar2=None, op0=ALU.mult),
             reads=["gpost"], writes=["gpost"])

        def prologue(t):
            hTt = hT[t % 2]
            for sub in range(4):
                r0 = t * TT + sub * 128
                xb = xst[sub % 2]
                hb = hst[sub % 2]
                xr, hr = "xst%d" % (sub % 2), "hst%d" % (sub % 2)
                P.dma("sp", xb[:], src[r0:r0 + 128, :], reads=["xs_r%d" % (r0 // 128)], writes=[xr])
                sc = (t * 4 + sub) % 8
                ss = stat[:, sc:sc + 1]
                P.op("act", lambda e, xb=xb, ss=ss: e.activation(out=junk[:], in_=xb[:], func=AF.Square, accum_out=ss),
                     reads=[xr], writes=["junk", "ss%d" % sc])
                if DBG == 21:
                    continue
                P.op("act", lambda e, ss=ss: e.activation(out=ss, in_=ss, func=AF.Sqrt, scale=1.0 / D, bias=self.eps),
                     reads=["eps", "ss%d" % sc], writes=["ss%d" % sc])
                P.op("dve", lambda e, ss=ss: e.reciprocal(out=ss, in_=ss), reads=["ss%d" % sc], writes=["ss%d" % sc])
                P.op("dve", lambda e, xb=xb, hb=hb, ss=ss: e.scalar_tensor_tensor(
                    out=hb[:], in0=xb[:], scalar=ss, in1=gpre[:], op0=ALU.mult, op1=ALU.mult),
                    reads=[xr, "ss%d" % sc, "gpre"], writes=[hr])
                if DBG == 22:
                    continue
                for c in range(8):
                    bk = 4 + c // 2
                    pv = bank[bk][:, :].bitcast(BF16)
                    col = (c % 2) * 512 + sub * 128
                    P.op("pe", lambda e, pv=pv, col=col, hb=hb, c=c: e.transpose(
                        out=pv[:, col:col + 128], in_=hb[:, c * 128:(c + 1) * 128], identity=self.ident[:]),
                        reads=[hr, "ident"], writes=["bank%d" % bk])
            if DBG in (21, 22, 23):
                return
            for c in range(8):
                bk = 4 + c // 2
                pv = bank[bk][:, :].bitcast(BF16)
                eng = "act" if bk % 2 == 0 else "dve"
                if eng == "act":
                    fn = lambda e, pv=pv, c=c: e.activation(out=hTt[:, c, :], in_=pv[:, (c % 2) * 512:(c % 2) * 512 + 512], func=AF.Copy)
                else:
                    fn = lambda e, pv=pv, c=c: e.tensor_copy(out=hTt[:, c, :], in_=pv[:, (c % 2) * 512:(c % 2) * 512 + 512])
                P.op(eng, fn, reads=["bank%d" % bk], writes=["hT%d_%d" % (t % 2, c)])

        def gate_up(t, c):
            hTt = hT[t % 2]
            bg, bu = (0, 1) if c % 2 == 0 else (2, 3)
            for kc in range(8):
                P.op("pe", lambda e, kc=kc: e.matmul(bank[bg][:, :], lhsT=self.wg[:, kc, c * 128:(c + 1) * 128],
                                                     rhs=hTt[:, kc, :], start=(kc == 0), stop=(kc == 7)),
                     reads=["wg%d" % c, "hT%d_%d" % (t % 2, kc)], writes=["bank%d" % bg])
            for kc in range(8):
                P.op("pe", lambda e, kc=kc: e.matmul(bank[bu][:, :], lhsT=self.wu[:, kc, c * 128:(c + 1) * 128],
                                                     rhs=hTt[:, kc, :], start=(kc == 0), stop=(kc == 7)),
                     reads=["wu%d" % c, "hT%d_%d" % (t % 2, kc)], writes=["bank%d" % bu])
            sgb = sg[c % 2]
            P.op("act", lambda e: e.activation(out=sgb[:], in_=bank[bg][:, :], func=AF.Silu),
                 reads=["bank%d" % bg], writes=["sg%d" % (c % 2)])
            P.op("dve", lambda e: e.tensor_tensor(out=uT[:, c, :], in0=bank[bu][:, :], in1=sgb[:], op=ALU.mult),
                 reads=["bank%d" % bu, "sg%d" % (c % 2)], writes=["uT%d" % c])

        def down(t, sub):
            b0 = 4 + 2 * (sub % 2)
            r0 = t * TT + sub * 128
            for half in range(2):
                bk = b0 + half
                for c in range(NFC):
                    P.op("pe", lambda e, c=c, bk=bk, half=half: e.matmul(
                        bank[bk][:, :], lhsT=uT[:, c, sub * 128:(sub + 1) * 128],
                        rhs=self.wd[:, c, half * 512:(half + 1) * 512], start=(c == 0), stop=(c == NFC - 1)),
                        reads=["uT%d" % c, "wd%d" % c], writes=["bank%d" % bk])
            sc = 8 + (t * 4 + sub) % 8
            ss2 = [stat[:, sc + 8 * hf:sc + 8 * hf + 1] for hf in range(2)]
            ss = stat[:, sc:sc + 1]
            for hf in range(2):
                P.op("act", lambda e, hf=hf: e.activation(out=junk[:, hf * 512:(hf + 1) * 512], in_=bank[b0 + hf][:, :],
                                                        func=AF.Square, accum_out=ss2[hf]),
                     reads=["bank%d" % (b0 + hf)], writes=["junk", "ssy%d_%d" % (sc, hf)])
            P.op("dve", lambda e: e.tensor_tensor(out=ss, in0=ss2[0], in1=ss2[1], op=ALU.add),
                 reads=["ssy%d_0" % sc, "ssy%d_1" % sc], writes=["ssy%d_0" % sc])
            P.op("act", lambda e: e.activation(out=ss, in_=ss, func=AF.Sqrt, scale=1.0 / D, bias=self.eps),
                 reads=["eps", "ssy%d_0" % sc], writes=["ssy%d_0" % sc])
            P.op("dve", lambda e: e.reciprocal(out=ss, in_=ss), reads=["ssy%d_0" % sc], writes=["ssy%d_0" % sc])
            tb = tst[sub % 2]
            xb = xst[sub % 2]
            tr, xr = "tst%d" % (sub % 2), "xst%d" % (sub % 2)
            P.dma("sp", xb[:], src[r0:r0 + 128, :], reads=["xs_r%d" % (r0 // 128)], writes=[xr])
            for hf in range(2):
                P.op("dve", lambda e, hf=hf: e.scalar_tensor_tensor(
                    out=tb[:, hf * 512:(hf + 1) * 512], in0=bank[b0 + hf][:, :], scalar=ss,
                    in1=gpost[:, hf * 512:(hf + 1) * 512], op0=ALU.mult, op1=ALU.mult),
                    reads=["bank%d" % (b0 + hf), "ssy%d_0" % sc, "gpost"], writes=[tr])
            P.op("dve", lambda e: e.tensor_tensor(out=tb[:], in0=tb[:], in1=xb[:], op=ALU.add),
                 reads=[tr, xr], writes=[tr])
            o = P.dma("sp", dst[r0:r0 + 128, :], tb[:], reads=[tr], writes=["xs_r%d" % (r0 // 128)])
            if is_out:
                self.out_dmas.append(o)

        if DBG == 1:
            return
        prologue(0)
        if DBG in (2, 21, 22, 23, 24, 25):
            return
        for t in range(NT):
            for c in range(NFC):
                gate_up(t, c)
                if c == 10 and t + 1 < NT:
                    prologue(t + 1)
            if DBG == 3:
                return
            for sub in range(4):
                down(t, sub)

    w_loaded = None

    def ffn_weights(self, l, which):
        P = self.P
        wgd = self.prm[which + "_w_gate"][l].rearrange("(kc p) f -> p kc f", p=128)
        wud = self.prm[which + "_w_up"][l].rearrange("(kc p) f -> p kc f", p=128)
        wdd = self.prm[which + "_w_down"][l].rearrange("(fc p) d -> p fc d", p=128)
        amb, P.ambient = P.ambient, []
        groups = [(i * 512, min(FF, (i + 1) * 512)) for i in range((FF + 511) // 512)]
        for gi, (a, b) in enumerate(groups):
            P.dma("pool", self.wg[:, :, a:b], wgd[:, :, a:b], writes=["wg%d" % c for c in range(a // 128, b // 128)])
            P.dma("pool", self.wu[:, :, a:b], wud[:, :, a:b], writes=["wu%d" % c for c in range(a // 128, b // 128)])
        for c0 in range(0, NFC, 2):
            P.dma("pool", self.wd[:, c0:c0 + 2, :], wdd[:, c0:c0 + 2, :], writes=["wd%d" % c0, "wd%d" % (c0 + 1)])
        P.ambient = amb
        self.w_loaded = (l, which)

    def barrier(self):
        self.P.op("pool", lambda e: e.memset(self.stat[:, 62:63], 0.0), writes=["regB"])

    def attn_phase(self, l, src, dst):
        if DBG == 41:
            return
        self.proj_phase(l, src)
        if DBG == 31 or DBG >= 41:
            return
        if self.stages > 3 * l + 2:
            self.ffn_weights(l, "ffn2")
        self.attn_core(l, moba=True)
        if DBG == 32:
            return
        self.attn_core(l, moba=False)
        self.outproj_phase(l, src, dst)

    def prologue_x(self, t, src, gpre, xst, hst, hTt, junk, htag, part="all"):
        P, bank, stat = self.P, self.bank, self.stat
        nh_ = len(hst)
        for sub in range(4):
            r0 = t * TT + sub * 128
            xb, hb = xst[sub % 2], hst[sub % nh_]
            xr, hr = "xst%d" % (sub % 2), "hst%d" % (sub % nh_)
            if part == "tr":
                for c in range(8):
                    bk = 4 + c // 2
                    pv = bank[bk][:, :].bitcast(BF16)
                    col = (c % 2) * 512 + sub * 128
                    P.op("pe", lambda e, pv=pv, col=col, hb=hb, c=c: e.transpose(
                        out=pv[:, col:col + 128], in_=hb[:, c * 128:(c + 1) * 128], identity=self.ident[:]),
                        reads=[hr, "ident"], writes=["bank%d" % bk])
                continue
            P.dma("sp", xb[:], src[r0:r0 + 128, :], reads=["xs_r%d" % (r0 // 128)], writes=[xr])
            sc = (t * 4 + sub) % 8
            ss = stat[:, sc:sc + 1]
            P.op("act", lambda e, xb=xb, ss=ss: e.activation(out=junk[:], in_=xb[:], func=AF.Square, accum_out=ss),
                 reads=[xr], writes=["junk", "ss%d" % sc])
            P.op("act", lambda e, ss=ss: e.activation(out=ss, in_=ss, func=AF.Sqrt, scale=1.0 / D, bias=self.eps),
                 reads=["eps", "ss%d" % sc], writes=["ss%d" % sc])
            P.op("dve", lambda e, ss=ss: e.reciprocal(out=ss, in_=ss), reads=["ss%d" % sc], writes=["ss%d" % sc])
            P.op("dve", lambda e, xb=xb, hb=hb, ss=ss: e.scalar_tensor_tensor(
                out=hb[:], in0=xb[:], scalar=ss, in1=gpre[:], op0=ALU.mult, op1=ALU.mult),
                reads=[xr, "ss%d" % sc, "gpre"], writes=[hr])
            if part == "norm":
                continue
            for c in range(8):
                bk = 4 + c // 2
                pv = bank[bk][:, :].bitcast(BF16)
                col = (c % 2) * 512 + sub * 128
                P.op("pe", lambda e, pv=pv, col=col, hb=hb, c=c: e.transpose(
                    out=pv[:, col:col + 128], in_=hb[:, c * 128:(c + 1) * 128], identity=self.ident[:]),
                    reads=[hr, "ident"], writes=["bank%d" % bk])
        if part == "norm":
            return
        for c in range(8):
            bk = 4 + c // 2
            pv = bank[bk][:, :].bitcast(BF16)
            a = (c % 2) * 512
            if bk % 2 == 0:
                P.op("act", lambda e, pv=pv, c=c, a=a: e.activation(out=hTt[:, c, :], in_=pv[:, a:a + 512], func=AF.Copy),
                     reads=["bank%d" % bk], writes=["%s_%d" % (htag, c)])
            else:
                P.op("dve", lambda e, pv=pv, c=c, a=a: e.tensor_copy(out=hTt[:, c, :], in_=pv[:, a:a + 512]),
                     reads=["bank%d" % bk], writes=["%s_%d" % (htag, c)])

    def epilogue_y(self, b0, r0, idx, src, dst, gpost, xst, tst, junk):
        P, bank, stat = self.P, self.bank, self.stat
        sc = 8 + idx % 8
        ss2 = [stat[:, sc + 8 * hf:sc + 8 * hf + 1] for hf in range(2)]
        ss = stat[:, sc:sc + 1]
        for hf in range(2):
            P.op("act", lambda e, hf=hf: e.activation(out=junk[:, hf * 512:(hf + 1) * 512], in_=bank[b0 + hf][:, :],
                                                    func=AF.Square, accum_out=ss2[hf]),
                 reads=["bank%d" % (b0 + hf)], writes=["junk", "ssy%d_%d" % (sc, hf)])
        P.op("dve", lambda e: e.tensor_tensor(out=ss, in0=ss2[0], in1=ss2[1], op=ALU.add),
             reads=["ssy%d_0" % sc, "ssy%d_1" % sc], writes=["ssy%d_0" % sc])
        P.op("act", lambda e: e.activation(out=ss, in_=ss, func=AF.Sqrt, scale=1.0 / D, bias=self.eps),
             reads=["eps", "ssy%d_0" % sc], writes=["ssy%d_0" % sc])
        P.op("dve", lambda e: e.reciprocal(out=ss, in_=ss), reads=["ssy%d_0" % sc], writes=["ssy%d_0" % sc])
        tb, xb = tst[idx % 2], xst[idx % 2]
        tr, xr = "tst%d" % (idx % 2), "xst%d" % (idx % 2)
        P.dma("sp", xb[:], src[r0:r0 + 128, :], reads=["xs_r%d" % (r0 // 128)], writes=[xr])
        for hf in range(2):
            P.op("dve", lambda e, hf=hf: e.scalar_tensor_tensor(
                out=tb[:, hf * 512:(hf + 1) * 512], in0=bank[b0 + hf][:, :], scalar=ss,
                in1=gpost[:, hf * 512:(hf + 1) * 512], op0=ALU.mult, op1=ALU.mult),
                reads=["bank%d" % (b0 + hf), "ssy%d_0" % sc, "gpost"], writes=[tr])
        P.op("dve", lambda e: e.tensor_tensor(out=tb[:], in0=tb[:], in1=xb[:], op=ALU.add), reads=[tr, xr], writes=[tr])
        o = P.dma("sp", dst[r0:r0 + 128, :], tb[:], reads=[tr], writes=["xs_r%d" % (r0 // 128)])
        if dst is self.out:
            self.out_dmas.append(o)

    def attn_consts(self):
        P = self.P
        NB = S // 256
        self.tri = self.sb("tri", [128, 128], BF16)
        self.btab = self.sb("btab", [128, 12, 32], F32)
        self.regB = self.sb_off
        tmpf = self.sb("tmpf", [128, 128], F32)
        t0 = self.sb("t0", [128, 32], F32)
        poshi = self.sb("poshi", [1, S], BF16)
        poslo = self.sb("poslo", [1, S], BF16)
        onehot = self.sb("onehot", [16, S], BF16)
        slr = [self.sb("slr", [2, S], BF16) for _ in range(4)]
        P.op("pool", lambda e: e.memset(tmpf[:], NEG), writes=["tmpf"])
        P.op("pool", lambda e: e.affine_select(out=tmpf[:], in_=tmpf[:], pattern=[[-1, 128]], compare_op=ALU.is_gt,
                                               fill=0.0, base=0, channel_multiplier=1), reads=["tmpf"], writes=["tmpf"])
        P.op("pool", lambda e: e.tensor_copy(out=self.tri[:], in_=tmpf[:]), reads=["tmpf"], writes=["tri"])
        P.op("pool", lambda e: e.iota(t0[:], pattern=[[-128, 32]], base=384, channel_multiplier=1,
                                      allow_small_or_imprecise_dtypes=True), writes=["t0"])
        self.slopes = [2.0 ** (-(i + 1)) for i in range(MH)] + [2.0 ** (-2 * (i + 1)) for i in range(DH)]
        for hh in range(12):
            P.op("pool", lambda e, hh=hh: e.tensor_scalar(out=self.btab[:, hh, :], in0=t0[:], scalar1=self.slopes[hh],
                                                         scalar2=None, op0=ALU.mult), reads=["t0"], writes=["btab"])
        P.op("pool", lambda e: e.iota(poshi[:], pattern=[[0, S // 512], [256, 2], [0, 256]], base=0, channel_multiplier=0,
                                      allow_small_or_imprecise_dtypes=True), writes=["poshi"])
        P.op("pool", lambda e: e.iota(poslo[:], pattern=[[0, S // 256], [1, 256]], base=0, channel_multiplier=0,
                                      allow_small_or_imprecise_dtypes=True), writes=["poslo"])
        P.op("pool", lambda e: e.memset(onehot[:], 1.0), writes=["onehot"])
        P.op("pool", lambda e: e.affine_select(out=onehot[:], in_=onehot[:], pattern=[[1, NB], [0, 256]], compare_op=ALU.is_equal,
                                               fill=0.0, base=0, channel_multiplier=-1), reads=["onehot"], writes=["onehot"])
        for h in range(MH):
            P.dma("sp", self.QA[h, 80:81, :], poshi[:], reads=["poshi"])
            P.dma("sp", self.QA[h, 81:82, :], poslo[:], reads=["poslo"])
            P.dma("sp", self.KA[h, 64:80, :], onehot[:], reads=["onehot"])
        for h in range(DH):
            for m in range(2):
                P.dma("sp", self.QD[h, m, 64:65, :], poshi[:], reads=["poshi"])
                P.dma("sp", self.QD[h, m, 65:66, :], poslo[:], reads=["poslo"])
        for hh in range(12):
            sl = slr[hh % 4]
            P.op("pool", lambda e, sl=sl, hh=hh: e.memset(sl[:], -8.0 * self.slopes[hh]), writes=["slr%d" % (hh % 4)])
            if hh < MH:
                P.dma("sp", self.KA[hh, 80:82, :], sl[:], reads=["slr%d" % (hh % 4)])
            else:
                for m in range(2):
                    P.dma("sp", self.KD[hh - MH, m, 64:66, :], sl[:], reads=["slr%d" % (hh % 4)])

    def proj_phase(self, l, src):
        P, bank, stat = self.P, self.bank, self.stat
        NB = S // 256
        self.barrier()
        self.sb_off = self.regB
        gpre = self.sb("gpre", [128, D], F32)
        xst = [self.sb("xst", [128, D], F32) for _ in range(2)]
        hst = [self.sb("hst", [128, D], BF16) for _ in range(4)]
        hT = [self.sb("hT", [128, 8, TT], BF16) for _ in range(2)]
        junk = self.sb("junk", [128, D], BF16)
        qst = [self.sb("qst", [128, TT], BF16) for _ in range(NQST)]
        vms = [self.sb("vms", [128, MH, 65], BF16) for _ in range(2)]
        vds = [self.sb("vds", [128, DH, 129], BF16) for _ in range(2)]
        kmf = self.sb("kmf", [128, 4, 16], F32)
        kmb = self.sb("kmb", [128, 4, 32], BF16)
        maskc = self.sb("maskc", [128, 16, 128], F32)
        mt1 = self.sb("mt1", [128, 16, 128], F32)
        gsb = [self.sb("gsb", [128, 128], F32) for _ in range(2)]
        mx8 = [self.sb("mx8", [128, MH, 8], F32) for _ in range(2)]
        selb = [self.sb("selb", [128, 128], BF16) for _ in range(4)]
        selT = [self.sb("selT", [128, TT], BF16) for _ in range(2)]
        win = self.nc.alloc_sbuf_tensor_at("win_%d" % l, [128, 8, INW], BF16, offset=SB_BASE)
        wind = self.prm["w_in"][l].rearrange("(kc p) f -> p kc f", p=128)
        allw = ["wg%d" % c for c in range(NFC)] + ["wu%d" % c for c in range(NFC)]
        for g in range(6):
            P.dma("pool", win[:, :, g * 512:(g + 1) * 512], wind[:, :, g * 512:(g + 1) * 512],
                  writes=(allw if g == 0 else []) + ["win%d" % g])
        P.dma("pool", gpre[:], self.prm["mix_pre_g"][l].partition_broadcast(128), writes=["gpre"])
        P.op("pool", lambda e: e.memset(kmf[:], 0.0), writes=["kmf"])
        P.op("pool", lambda e: e.memset(kmb[:], 0.0), writes=["kmb"])
        for b_ in range(2):
            P.op("pool", lambda e, b_=b_: e.memset(vms[b_][:, :, 64:65], 1.0), writes=["vms%d" % b_])
            P.op("pool", lambda e, b_=b_: e.memset(vds[b_][:, :, 128:129], 1.0), writes=["vds%d" % b_])
        P.op("pool", lambda e: e.iota(mt1[:], pattern=[[-1, 16], [0, MH], [1, 16]], base=0, channel_multiplier=0,
                                      allow_small_or_imprecise_dtypes=True), writes=["mt1"])
        P.op("pool", lambda e: e.tensor_scalar(out=maskc[:], in0=mt1[:], scalar1=0.0, scalar2=-1e30, op0=ALU.is_gt, op1=ALU.mult),
             reads=["mt1"], writes=["maskc"])
        P.op("pool", lambda e: e.tensor_scalar(out=mt1[:], in0=mt1[:], scalar1=0.0, scalar2=1e30, op0=ALU.is_equal, op1=ALU.mult),
             reads=["mt1", "maskc"], writes=["mt1"])
        P.op("pool", lambda e: e.tensor_tensor(out=maskc[:], in0=maskc[:], in1=mt1[:], op=ALU.add),
             reads=["mt1", "maskc"], writes=["maskc"])
        wr = ["wg0"]
        cnt = {"pb": 0, "q": 0}
        if DBG == 42:
            return

        def fm_chunk(t, oc):
            bk = cnt["pb"] % 2
            cnt["pb"] += 1
            for kc in range(8):
                P.op("pe", lambda e, kc=kc, bk=bk: e.matmul(bank[bk][:, :], lhsT=win[:, kc, oc * 128:(oc + 1) * 128],
                                                           rhs=hT[t % 2][:, kc, :], start=(kc == 0), stop=(kc == 7)),
                     reads=["win%d" % (oc // 4), "hTp%d_%d" % (t % 2, kc)] + wr, writes=["bank%d" % bk])
            return bk

        def evac_q(bk):
            qi = cnt["q"] % NQST
            cnt["q"] += 1
            P.op("act", lambda e: e.activation(out=qst[qi][:], in_=bank[bk][:, :], func=AF.Copy),
                 reads=["bank%d" % bk], writes=["qst%d" % qi])
            return qi

        cols = lambda t: slice(t * TT, (t + 1) * TT)
        self.prologue_x(0, src, gpre, xst, hst, hT[0], junk, "hTp0")
        for t in range(NT):
            if t + 1 < NT:
                self.prologue_x(t + 1, src, gpre, xst, hst, hT[(t + 1) % 2], junk, "hTp%d" % ((t + 1) % 2), part="norm")
            for oc in range(4, 8):
                bk = fm_chunk(t, oc)
                qi = evac_q(bk)
                P.op("dve", lambda e, bk=bk, oc=oc, t=t: e.tensor_reduce(
                    out=kmf[:, oc - 4, 2 * t:2 * t + 2], in_=bank[bk][:, :].rearrange("p (a b) -> p a b", a=2),
                    axis=AX.X, op=ALU.add), reads=["bank%d" % bk, "kmf"], writes=["kmf"])
                for hh in range(2):
                    P.dma("sp", self.KA[2 * (oc - 4) + hh, 0:64, cols(t)], qst[qi][hh * 64:(hh + 1) * 64, :],
                          reads=["qst%d" % qi], writes=["KA"])
            for hh in range(2):
                P.op("dve", lambda e, hh=hh: e.tensor_scalar(
                    out=kmb[hh * 64:(hh + 1) * 64, :, hh * 16:(hh + 1) * 16], in0=kmf[hh * 64:(hh + 1) * 64, :, :],
                    scalar1=1.0 / 256, scalar2=None, op0=ALU.mult), reads=["kmf"], writes=["kmb"])
            for oc in range(0, 4):
                bk = fm_chunk(t, oc)
                qi = evac_q(bk)
                for hh in range(2):
                    P.dma("sp", self.QA[2 * oc + hh, 0:64, cols(t)], qst[qi][hh * 64:(hh + 1) * 64, :],
                          reads=["qst%d" % qi], writes=["QA"])
                for s_ in range(4):
                    P.op("pe", lambda e, s_=s_, qi=qi, oc=oc: e.matmul(
                        bank[3][:, s_ * 128 + oc * 32:s_ * 128 + oc * 32 + 32],
                        lhsT=qst[qi][:, s_ * 128:(s_ + 1) * 128],
                        rhs=kmb[:, oc, :], start=True, stop=True, skip_group_check=True),
                        reads=["qst%d" % qi, "kmb"], writes=["bank3"])
            pvT = bank[2][:, :].bitcast(BF16)
            for s_ in range(4):
                cur = 2 * t + s_ // 2
                gb, mb, sbb = gsb[s_ % 2], mx8[s_ % 2], selb[s_]
                gr, mr, sr = "gsb%d" % (s_ % 2), "mx8%d" % (s_ % 2), "selb%d" % s_
                P.op("dve", lambda e, s_=s_, cur=cur, gb=gb: e.tensor_tensor(
                    out=gb[:], in0=bank[3][:, s_ * 128:(s_ + 1) * 128], in1=maskc[:, cur, :], op=ALU.add),
                    reads=["bank3", "maskc"], writes=[gr])
                for h in range(MH):
                    P.op("dve", lambda e, h=h, gb=gb, mb=mb: e.max(out=mb[:, h, :], in_=gb[:, h * 16:(h + 1) * 16]),
                         reads=[gr], writes=[mr])
                for h in range(MH):
                    P.op("dve", lambda e, h=h, gb=gb, mb=mb, sbb=sbb: e.tensor_scalar(
                        out=sbb[:, h * 16:(h + 1) * 16], in0=gb[:, h * 16:(h + 1) * 16], scalar1=mb[:, h, 3:4], scalar2=NEG,
                        op0=ALU.is_lt, op1=ALU.mult), reads=[gr, mr], writes=[sr])

            def sel_tail(t=t, pvT=pvT):
                for s_ in range(4):
                    sbb, sr = selb[s_], "selb%d" % s_
                    P.op("pe", lambda e, s_=s_, sbb=sbb: e.transpose(out=pvT[:, s_ * 128:(s_ + 1) * 128], in_=sbb[:], identity=self.ident[:]),
                         reads=[sr, "ident"], writes=["bank2"])
                sT = selT[t % 2]
                P.op("act", lambda e, sT=sT: e.activation(out=sT[:], in_=pvT[:, 0:512], func=AF.Copy),
                     reads=["bank2"], writes=["selT%d" % (t % 2)])
                for h in range(MH):
                    P.dma("sp", self.QA[h, 64:80, cols(t)], sT[h * 16:(h + 1) * 16, :], reads=["selT%d" % (t % 2)], writes=["QA"])
            if DBG == 44:
                continue
            for oc in range(12, 20):
                bk = fm_chunk(t, oc)
                qi = evac_q(bk)
                dstT = self.QD if oc < 16 else self.KD
                hd = (oc - 12) % 4
                for m in range(2):
                    P.dma("sp", dstT[hd, m, 0:64, cols(t)], qst[qi][m * 64:(m + 1) * 64, :],
                          reads=["qst%d" % qi], writes=["QD"])
            if DBG == 45:
                continue
            for s_ in range(4):
                r0 = t * TT + s_ * 128
                for which in range(2):
                    bk = cnt["pb"] % 2
                    cnt["pb"] += 1
                    c0 = 1024 if which == 0 else 2560
                    for kc in range(8):
                        P.op("pe", lambda e, kc=kc, bk=bk, c0=c0, s_=s_, t=t: e.matmul(
                            bank[bk][:, :], lhsT=hT[t % 2][:, kc, s_ * 128:(s_ + 1) * 128], rhs=win[:, kc, c0:c0 + 512],
                            start=(kc == 0), stop=(kc == 7)),
                            reads=["win%d" % (c0 // 512), "hTp%d_%d" % (t % 2, kc)] + wr, writes=["bank%d" % bk])
                    if which == 0:
                        vb = vms[s_ % 2]
                        P.op("act", lambda e, bk=bk, vb=vb: e.activation(
                            out=vb[:, :, 0:64], in_=bank[bk][:, :].rearrange("p (h d) -> p h d", h=MH), func=AF.Copy),
                            reads=["bank%d" % bk], writes=["vms%d" % (s_ % 2)])
                        P.dma("sp", self.VM[r0:r0 + 128, :], vb[:].rearrange("p h d -> p (h d)"),
                              reads=["vms%d" % (s_ % 2)], writes=["VM"])
                    else:
                        vb = vds[s_ % 2]
                        P.op("dve", lambda e, bk=bk, vb=vb: e.tensor_copy(
                            out=vb[:, :, 0:128], in_=bank[bk][:, :].rearrange("p (h d) -> p h d", h=DH)),
                            reads=["bank%d" % bk], writes=["vds%d" % (s_ % 2)])
                        P.dma("sp", self.VD[r0:r0 + 128, :], vb[:].rearrange("p h d -> p (h d)"),
                              reads=["vds%d" % (s_ % 2)], writes=["VD"])
            sel_tail()
            if t + 1 < NT:
                self.prologue_x(t + 1, src, gpre, xst, hst, hT[(t + 1) % 2], junk, "hTp%d" % ((t + 1) % 2), part="tr")

    def attn_core(self, l, moba):
        P, bank, stat = self.P, self.bank, self.stat
        NKT = S // 128
        self.barrier()
        self.sb_off = self.regB
        nh = MH if moba else DH
        nm = 1 if moba else 2
        K = 82 if moba else 66
        dv = 64 if moba else 128
        vw = nh * (dv + 1)
        Vs = self.sb("Vs", [128, NKT, vw], BF16)
        Vd = (self.VM if moba else self.VD).rearrange("(i p) c -> p i c", p=128)
        step = max(1, NKT // 4)
        for i0 in range(0, NKT, step):
            P.dma("sp", Vs[:, i0:i0 + step, :], Vd[:, i0:i0 + step, :], reads=["VM", "VD"], writes=["Vs"])
        nqb = 2 if moba else 1
        Qb = [[self.sb("Qb", [128, S], BF16) for m in range(nm)] for _ in range(nqb)]
        Kb = [[self.sb("Kb", [128, S], BF16) for m in range(nm)] for _ in range(nqb)]
        for a_ in range(nqb):
            for m in range(nm):
                P.op("pool", lambda e, a_=a_, m=m: e.memset(Qb[a_][m][:], 0.0), writes=["Qb%d_%d" % (a_, m), "QKz"])
                P.op("pool", lambda e, a_=a_, m=m: e.memset(Kb[a_][m][:], 0.0), writes=["Kb%d_%d" % (a_, m), "QKz"])
        npt = 4 if moba else 3
        pt = [self.sb("pt", [128, 512], BF16) for _ in range(npt)]
        ostg = [self.sb("ostg", [128, 4, dv], BF16) for _ in range(2)]
        rz = self.sb("rz", [128, 16], F32)
        if not moba:
            lams = self.sb("lams", [128, 8], F32)
            subg = self.sb("subg", [128, 128], F32)
            t1off = self.sb_off
            t1 = [self.sb("t1", [128, 128], F32) for _ in range(2)]
            lamt = self.sb("lamt", [128, 4, 64], F32, off=t1off)
            asb = [self.sb("asb", [128, 128], F32) for _ in range(2)]
            junk = self.sb("junk", [128, 128], BF16)
            lam_init = 0.8 - 0.6 * math.exp(-0.3 * l)
            for i, nm_ in enumerate(["lambda_q1", "lambda_k1", "lambda_q2", "lambda_k2"]):
                P.dma("sp", lamt[:, i, :], self.prm[nm_][l].partition_broadcast(128), writes=["lamt"])
            P.dma("sp", subg[:], self.prm["subln_g"][l].partition_broadcast(128), writes=["subg"])
            P.op("dve", lambda e: e.tensor_scalar(out=subg[:], in0=subg[:], scalar1=1.0 - lam_init, scalar2=None, op0=ALU.mult),
                 reads=["subg"], writes=["subg"])
            for i in range(2):
                P.op("dve", lambda e, i=i: e.tensor_tensor(out=lamt[:, 2 * i, :], in0=lamt[:, 2 * i, :], in1=lamt[:, 2 * i + 1, :], op=ALU.mult),
                     reads=["lamt"], writes=["lamt"])
                P.op("dve", lambda e, i=i: e.tensor_reduce(out=lams[:, i:i + 1], in_=lamt[:, 2 * i, :], axis=AX.X, op=ALU.add),
                     reads=["lamt"], writes=["lams"])
                P.op("act", lambda e, i=i: e.activation(out=lams[:, i:i + 1], in_=lams[:, i:i + 1], func=AF.Exp),
                     reads=["lams"], writes=["lams"])
            P.op("dve", lambda e: e.tensor_tensor(out=lams[:, 2:3], in0=lams[:, 1:2], in1=lams[:, 0:1], op=ALU.subtract),
                 reads=["lams"], writes=["lams"])
            P.op("dve", lambda e: e.tensor_scalar(out=lams[:, 2:3], in0=lams[:, 2:3], scalar1=-lam_init, scalar2=None, op0=ALU.add),
                 reads=["lams"], writes=["lams"])
            neglam = lams[:, 2:3]
        sbanks = [0, 1, 2] if moba else [0, 1]
        osets = [[4], [5], [6], [7]] if moba else [[2, 3, 4], [5, 6, 7]]
        NQ = S // 512
        units = []
        for h in range(nh):
            for j in range(NQ):
                oset = osets[(h * NQ + j) % len(osets)]
                for i in range(4 * j + 4):
                    for m in range(nm):
                        units.append(dict(h=h, j=j, i=i, m=m, oset=oset, first=(j == 0 and i == 0 and m == 0),
                                          last=(i == 4 * j + 3 and m == nm - 1)))
        started = {}

        def acc(u, s_, m):
            if moba:
                return u["oset"][0], s_ * 65
            a = s_ * 2 + m
            return u["oset"][a // 3], (a % 3) * 129

        def load_head(h):
            qb = h % nqb
            for m in range(nm):
                qsrc = self.QA[h] if moba else self.QD[h, m]
                ksrc = self.KA[h] if moba else self.KD[h, m]
                P.dma("sp", Qb[qb][m][0:K, :], qsrc, reads=["QA", "QD"], writes=["Qb%d_%d" % (qb, m)])
                P.dma("sp", Kb[qb][m][0:K, :], ksrc, reads=["KA", "KD"], writes=["Kb%d_%d" % (qb, m)])

        def stageA(n):
            u = units[n]
            h, j, i, m = u["h"], u["j"], u["i"], u["m"]
            if u["first"]:
                if h == 0 or nqb == 1:
                    load_head(h)
                if nqb == 2 and h + 1 < nh:
                    load_head(h + 1)
            qb = h % nqb
            hh = h if moba else MH + h
            d = i - 4 * j
            qlo = 128 * d if d > 0 else 0
            sb_ = sbanks[n % len(sbanks)]
            pb = n % npt
            P.op("pe", lambda e: e.matmul(
                bank[sb_][:, qlo:512], lhsT=Kb[qb][m][0:K, i * 128:(i + 1) * 128],
                rhs=Qb[qb][m][0:K, j * 512 + qlo:(j + 1) * 512], start=True, stop=(d < 0)),
                reads=["Kb%d_%d" % (qb, m), "Qb%d_%d" % (qb, m), "QKz"], writes=["bank%d" % sb_])
            if d >= 0:
                P.op("pe", lambda e: e.matmul(
                    bank[sb_][:, qlo:qlo + 128], lhsT=self.ident[:], rhs=self.tri[:], start=False, stop=True),
                    reads=["ident", "tri"], writes=["bank%d" % sb_])
            P.op("act", lambda e: e.activation(
                out=pt[pb][:, qlo:512], in_=bank[sb_][:, qlo:512], func=AF.Exp, scale=0.125,
                bias=self.btab[:, hh, 4 * j - i + 3:4 * j - i + 4]),
                reads=["bank%d" % sb_, "btab"], writes=["pt%d" % pb])

        def stageB(n):
            u = units[n]
            h, j, i, m = u["h"], u["j"], u["i"], u["m"]
            d = i - 4 * j
            pb = n % npt
            key = (h, j)
            st_set = started.setdefault(key, set())
            for s_ in range(max(d, 0), 4):
                bk, c0 = acc(u, s_, m)
                st = bk not in st_set
                st_set.add(bk)
                P.op("pe", lambda e, bk=bk, c0=c0, s_=s_, st=st: e.matmul(
                    bank[bk][:, c0:c0 + dv + 1], lhsT=pt[pb][:, s_ * 128:(s_ + 1) * 128],
                    rhs=Vs[:, i, h * (dv + 1):(h + 1) * (dv + 1)], start=st, stop=(i == 4 * j + s_),
                    skip_group_check=True),
                    reads=["pt%d" % pb, "Vs"], writes=["bank%d" % bk])
            if u["last"]:
                epilogue(u)

        ecount = {"n": 0}

        def epilogue(u):
            h, j = u["h"], u["j"]
            en = ecount["n"]
            ecount["n"] += 1
            og = ostg[en % 2]
            orr = "ostg%d" % (en % 2)
            if moba:
                bk = u["oset"][0]
                ov = bank[bk][:, 0:260].rearrange("p (s c) -> p s c", s=4)
                P.op("dve", lambda e: e.reciprocal(out=rz[:, 0:4], in_=ov[:, :, 64]), reads=["bank%d" % bk], writes=["rz"])
                for s_ in range(4):
                    P.op("dve", lambda e, s_=s_: e.tensor_scalar(
                        out=og[:, s_, :], in0=ov[:, s_, 0:64], scalar1=rz[:, s_:s_ + 1], scalar2=None, op0=ALU.mult),
                        reads=["bank%d" % bk, "rz"], writes=[orr])
                P.dma("sp", self.MIX[j * 512:(j + 1) * 512, h * 64:(h + 1) * 64].rearrange("(s p) c -> p s c", p=128),
                      og[:], reads=[orr], writes=["MIX"])
                return
            for s_ in range(4):
                b1, c1 = acc(u, s_, 0)
                b2, c2 = acc(u, s_, 1)
                tt, aa = t1[s_ % 2], asb[s_ % 2]
                tr, ar = "t1%d" % (s_ % 2), "asb%d" % (s_ % 2)
                r1, r2, ssq = rz[:, 4 * s_:4 * s_ + 1], rz[:, 4 * s_ + 1:4 * s_ + 2], rz[:, 4 * s_ + 2:4 * s_ + 3]
                rr = "rz%d" % s_
                P.op("dve", lambda e, b1=b1, c1=c1, r1=r1: e.reciprocal(out=r1, in_=bank[b1][:, c1 + 128:c1 + 129]),
                     reads=["bank%d" % b1], writes=[rr])
                P.op("dve", lambda e, b2=b2, c2=c2, r2=r2: e.reciprocal(out=r2, in_=bank[b2][:, c2 + 128:c2 + 129]),
                     reads=["bank%d" % b2, rr], writes=[rr])
                P.op("dve", lambda e, r2=r2: e.tensor_tensor(out=r2, in0=r2, in1=neglam, op=ALU.mult),
                     reads=[rr, "lams"], writes=[rr])
                P.op("dve", lambda e, b1=b1, c1=c1, r1=r1, tt=tt: e.tensor_scalar(
                    out=tt[:], in0=bank[b1][:, c1:c1 + 128], scalar1=r1, scalar2=None, op0=ALU.mult),
                    reads=["bank%d" % b1, rr], writes=[tr])
                P.op("dve", lambda e, b2=b2, c2=c2, r2=r2, tt=tt, aa=aa: e.scalar_tensor_tensor(
                    out=aa[:], in0=bank[b2][:, c2:c2 + 128], scalar=r2, in1=tt[:], op0=ALU.mult, op1=ALU.add),
                    reads=["bank%d" % b2, rr, tr], writes=[ar])
                P.op("act", lambda e, aa=aa, ssq=ssq: e.activation(out=junk[:], in_=aa[:], func=AF.Square, accum_out=ssq),
                     reads=[ar, rr], writes=["junkd", rr])
                P.op("act", lambda e, ssq=ssq: e.activation(out=ssq, in_=ssq, func=AF.Sqrt, scale=1.0 / 128, bias=self.eps),
                     reads=["eps", rr], writes=[rr])
                P.op("dve", lambda e, ssq=ssq: e.reciprocal(out=ssq, in_=ssq), reads=[rr], writes=[rr])
                P.op("dve", lambda e, aa=aa, ssq=ssq, s_=s_: e.scalar_tensor_tensor(
                    out=og[:, s_, :], in0=aa[:], scalar=ssq, in1=subg[:], op0=ALU.mult, op1=ALU.mult),
                    reads=[ar, rr, "subg"], writes=[orr])
            P.dma("sp", self.MIX[j * 512:(j + 1) * 512, 512 + h * 128:512 + (h + 1) * 128].rearrange("(s p) c -> p s c", p=128),
                  og[:], reads=[orr], writes=["MIX"])

        LA = 2 if moba else 1
        for n in range(min(LA, len(units))):
            stageA(n)
        for n in range(len(units)):
            if n + LA < len(units):
                stageA(n + LA)
            stageB(n)


    def outproj_phase(self, l, src, dst):
        P, bank, stat = self.P, self.bank, self.stat
        self.barrier()
        self.sb_off = self.regB
        wo = self.sb("wo", [128, 8, D], BF16)
        gpost = self.sb("gpost", [128, D], F32)
        mst = [self.sb("mst", [128, D], BF16) for _ in range(3)]
        mT = [self.sb("mT", [128, 8, 128], BF16) for _ in range(2)]
        xst = [self.sb("xst", [128, D], F32) for _ in range(2)]
        tst = [self.sb("tst", [128, D], F32) for _ in range(2)]
        junk = self.sb("junk", [128, D], BF16)
        wod = self.prm["w_out"][l].rearrange("(kc p) f -> p kc f", p=128)
        for g in range(2):
            P.dma("pool", wo[:, :, g * 512:(g + 1) * 512], wod[:, :, g * 512:(g + 1) * 512], writes=["wo%d" % g])
        P.dma("pool", gpost[:], self.prm["mix_post_g"][l].partition_broadcast(128), writes=["gpost"])
        def load_mix(r):
            P.dma("sp", mst[r % 3][:], self.MIX[r * 128:(r + 1) * 128, :], reads=["MIX"], writes=["mst%d" % (r % 3)])

        load_mix(0)
        load_mix(1)
        for r in range(S // 128):
            r0 = r * 128
            mb, mtb = mst[r % 3], mT[r % 2]
            if r + 2 < S // 128:
                load_mix(r + 2)
            tb_ = r % 2
            pv = bank[tb_][:, :].bitcast(BF16)
            for c in range(8):
                P.op("pe", lambda e, pv=pv, c=c, mb=mb: e.transpose(out=pv[:, c * 128:(c + 1) * 128], in_=mb[:, c * 128:(c + 1) * 128],
                                                                  identity=self.ident[:]),
                     reads=["mst%d" % (r % 3), "ident"], writes=["bank%d" % tb_])
            if tb_ == 0:
                P.op("act", lambda e, pv=pv, mtb=mtb: e.activation(out=mtb[:].rearrange("p c t -> p (c t)"), in_=pv[:, :], func=AF.Copy),
                     reads=["bank%d" % tb_], writes=["mT%d" % (r % 2)])
            else:
                P.op("dve", lambda e, pv=pv, mtb=mtb: e.tensor_copy(out=mtb[:].rearrange("p c t -> p (c t)"), in_=pv[:, :]),
                     reads=["bank%d" % tb_], writes=["mT%d" % (r % 2)])
            b0 = 4 + 2 * (r % 2)
            for hf in range(2):
                for c in range(8):
                    P.op("pe", lambda e, c=c, hf=hf, mtb=mtb, b0=b0: e.matmul(bank[b0 + hf][:, :], lhsT=mtb[:, c, :],
                                                                     rhs=wo[:, c, hf * 512:(hf + 1) * 512], start=(c == 0), stop=(c == 7)),
                         reads=["mT%d" % (r % 2), "wo%d" % hf], writes=["bank%d" % (b0 + hf)])
            self.epilogue_y(b0, r0, r, src, dst, gpost, xst, tst, junk)


_NC_CACHE = {}


def get_nc(stages):
    if stages not in _NC_CACHE:
        _NC_CACHE[stages] = Builder(stages).build()
    return _NC_CACHE[stages]


STAGES = 6
DBG = 0


def kernel(**inputs):
    nc = get_nc(STAGES)
    x = np.ascontiguousarray(inputs["x"], dtype=np.float32)
    shared = {k: np.ascontiguousarray(v, dtype=np.float32) for k, v in inputs.items() if k != "x"}
    in_maps = []
    for b in range(8):
        m = dict(shared)
        m["x"] = x[b]
        in_maps.append(m)
    res = run_bass_kernel_spmd(nc, in_maps, core_ids=list(range(8)))
    return np.stack([res.results[b]["out"] for b in range(8)], axis=0)
```

```python
import math
import contextlib
import numpy as np
import concourse.bass as bass
import concourse.mybir as mybir
from concourse.bass_utils import run_bass_kernel_spmd

F32 = mybir.dt.float32
BF16 = mybir.dt.bfloat16
AF = mybir.ActivationFunctionType
ALU = mybir.AluOpType
AX = mybir.AxisListType

D = 1024
S = 4096
L = 2
FF = 2816
NFC = FF // 128
HD = 64
MH = 8
DH = 4
INW = 3072
EPS = 1e-6
TT = 512
NT = S // TT
NEG = -30000.0
NQST = 6


class Res:
    __slots__ = ("name", "last_write", "reads")

    def __init__(self, name):
        self.name = name
        self.last_write = None
        self.reads = []


class Op:
    __slots__ = ("eng", "fn", "seq", "waits", "signal", "sig", "is_dma", "dsem", "dval", "dprev")

    def __init__(self, eng, fn, is_dma=False):
        self.eng = eng
        self.fn = fn
        self.seq = 0
        self.waits = {}
        self.signal = False
        self.sig = 0
        self.is_dma = is_dma
        self.dsem = None
        self.dval = 0
        self.dprev = None


class Prog:
    ENGS = ("pe", "act", "dve", "pool", "sp")

    def __init__(self, nc, ndma_sems=12):
        self.nc = nc
        self.ops = {e: [] for e in self.ENGS}
        self.known = {e: {} for e in self.ENGS}
        self.dma_count = {e: 0 for e in self.ENGS}
        self.dma_ops = {e: [] for e in self.ENGS}
        self.ndma = ndma_sems
        self.res = {}
        self.ambient = []

    def R(self, name):
        r = self.res.get(name)
        if r is None:
            r = self.res[name] = Res(name)
        return r

    def _dep(self, a, b):
        if a is None or a is b:
            return
        if a.is_dma:
            key = ("dma",) + a.dsem
            if self.known[b.eng].get(key, 0) >= a.dval:
                return
            self.known[b.eng][key] = a.dval
            b.waits[key] = a
            return
        if a.eng == b.eng and a.eng == "pe":
            return
        k = self.known[b.eng].get(a.eng, 0)
        if a.seq <= k:
            return
        cur = b.waits.get(a.eng)
        if cur is None or cur.seq < a.seq:
            b.waits[a.eng] = a
        a.signal = True

    def op(self, eng, fn, reads=(), writes=(), is_dma=False):
        o = Op(eng, fn, is_dma)
        lst = self.ops[eng]
        lst.append(o)
        o.seq = len(lst)
        writes = list(writes) + [r for r in reads if isinstance(r, str) and r.startswith("bank")]
        reads = [r for r in reads if not (isinstance(r, str) and r.startswith("bank"))]
        reads = [self.R(r) if isinstance(r, str) else r for r in list(reads) + self.ambient]
        writes = [self.R(w) if isinstance(w, str) else w for w in writes]
        for r in reads:
            self._dep(r.last_write, o)
        for w in writes:
            self._dep(w.last_write, o)
            for rd in w.reads:
                self._dep(rd, o)
        for r in reads:
            r.reads.append(o)
        for w in writes:
            w.last_write = o
            w.reads = []
        for k, a in o.waits.items():
            if not isinstance(k, tuple):
                self.known[eng][k] = max(self.known[eng].get(k, 0), a.seq)
        if is_dma:
            i = self.dma_count[eng]
            self.dma_count[eng] = i + 1
            o.dsem = (eng, i % self.ndma)
            o.dval = 16 * (i // self.ndma + 1)
            if i >= self.ndma:
                o.dprev = self.dma_ops[eng][i - self.ndma]
            self.dma_ops[eng].append(o)
        return o

    def dma(self, eng, out, in_, reads=(), writes=(), **kw):
        return self.op(eng, lambda e: e.dma_start(out=out, in_=in_, **kw), reads, writes, is_dma=True)

    def emit(self, final_waits=()):
        nc = self.nc
        engobj = {"pe": "tensor", "act": "scalar", "dve": "vector", "pool": "gpsimd", "sp": "sync"}
        with contextlib.ExitStack() as st:
            sems = {e: st.enter_context(nc.semaphore("s_" + e)) for e in self.ENGS}
            dsems = {}
            for e in self.ENGS:
                for k in range(min(self.ndma, self.dma_count[e])):
                    dsems[(e, k)] = st.enter_context(nc.semaphore("d_%s_%d" % (e, k)))
            for e in self.ENGS:
                c = 0
                for o in self.ops[e]:
                    if o.signal and not o.is_dma:
                        c += 1
                        o.sig = c
            block = st.enter_context(nc.Block())

            def run(e):
                def body(eng):
                    for o in self.ops[e]:
                        if o.dprev is not None:
                            eng.wait_ge(dsems[o.dprev.dsem], o.dprev.dval)
                        for k, a in o.waits.items():
                            if a.is_dma:
                                eng.wait_ge(dsems[a.dsem], a.dval)
                            else:
                                eng.wait_ge(sems[a.eng], a.sig)
                        ins = o.fn(eng)
                        if o.is_dma:
                            ins.then_inc(dsems[o.dsem], 16)
                        elif o.signal:
                            ins.then_inc(sems[e], 1)
                    if e == "sp":
                        for a in final_waits:
                            eng.wait_ge(dsems[a.dsem], a.dval)
                return body

            for e in self.ENGS:
                getattr(block, engobj[e])(run(e))


SB_BASE = 16512
SB_END = 229376


class Builder:
    def __init__(self, stages):
        self.stages = stages
        nc = self.nc = bass.Bass("TRN2", target_bir_lowering=False)
        self.P = Prog(nc)
        self.sb_off = SB_BASE
        self.uid = 0
        dt = lambda name, shape, dtype=F32, kind="ExternalInput": nc.dram_tensor(name, shape, dtype, kind=kind).ap()
        self.x_in = dt("x", [S, D])
        self.prm = {}
        for name, shape in [
            ("ffn1_pre_g", [L, D]), ("ffn1_w_gate", [L, D, FF]), ("ffn1_w_up", [L, D, FF]), ("ffn1_w_down", [L, FF, D]),
            ("ffn1_post_g", [L, D]), ("mix_pre_g", [L, D]), ("w_in", [L, D, INW]),
            ("lambda_q1", [L, HD]), ("lambda_k1", [L, HD]), ("lambda_q2", [L, HD]), ("lambda_k2", [L, HD]),
            ("subln_g", [L, 128]), ("w_out", [L, D, D]), ("mix_post_g", [L, D]),
            ("ffn2_pre_g", [L, D]), ("ffn2_w_gate", [L, D, FF]), ("ffn2_w_up", [L, D, FF]), ("ffn2_w_down", [L, FF, D]),
            ("ffn2_post_g", [L, D]),
        ]:
            self.prm[name] = dt(name, shape)
        self.out = dt("out", [S, D], F32, "ExternalOutput")
        self.xs = dt("xs", [S, D], F32, "Internal")
        self.bank = [nc.alloc_psum_tensor("bank%d" % i, [128, 512], F32) for i in range(8)]
        self.out_dmas = []
        self.QA = dt("QA", [MH, 82, S], BF16, "Internal")
        self.KA = dt("KA", [MH, 82, S], BF16, "Internal")
        self.VM = dt("VM", [S, MH * 65], BF16, "Internal")
        self.QD = dt("QD", [DH, 2, 66, S], BF16, "Internal")
        self.KD = dt("KD", [DH, 2, 66, S], BF16, "Internal")
        self.VD = dt("VD", [S, DH * 129], BF16, "Internal")
        self.MIX = dt("MIX", [S, D], BF16, "Internal")

    def sb(self, name, shape, dtype, off=None):
        esz = 2 if dtype == BF16 else 4
        n = esz
        for s_ in shape[1:]:
            n *= s_
        n = (n + 31) // 32 * 32
        if off is None:
            off = self.sb_off
            self.sb_off += n
            assert self.sb_off <= SB_END, (name, self.sb_off)
        self.uid += 1
        return self.nc.alloc_sbuf_tensor_at("%s_%d" % (name, self.uid), shape, dtype, offset=off)

    def build(self):
        P = self.P
        nc = self.nc
        self.wg = self.sb("wg", [128, 8, FF], BF16)
        self.wu = self.sb("wu", [128, 8, FF], BF16)
        self.wd = self.sb("wd", [128, NFC, D], BF16)
        self.ident = self.sb("ident", [128, 128], BF16)
        self.identf = self.sb("identf", [128, 128], F32)
        self.stat = self.sb("stat", [128, 64], F32)
        self.regB = self.sb_off
        self.eps = self.stat[:, 63:64]
        P.op("pool", lambda e: e.memset(self.eps, EPS), writes=["eps"])
        P.ambient = ["regB"]
        onesf = self.identf
        P.op("pool", lambda e: e.memset(onesf[:], 1.0), writes=["identf"])
        P.op("pool", lambda e: e.affine_select(out=onesf[:], in_=onesf[:], pattern=[[-1, 128]], compare_op=ALU.is_equal,
                                               fill=0.0, base=0, channel_multiplier=1), reads=["identf"], writes=["identf"])
        P.op("pool", lambda e: e.tensor_copy(out=self.ident[:], in_=onesf[:]), reads=["identf"], writes=["ident"])
        if self.stages > 1:
            self.attn_consts()

        src = self.x_in
        nst = 0
        for l in range(L):
            for which in ("ffn1", "attn", "ffn2"):
                if nst >= self.stages:
                    break
                nst += 1
                last = (nst == self.stages) or (l == L - 1 and which == "ffn2")
                dst = self.out if last else self.xs
                if which == "attn":
                    self.attn_phase(l, src, dst)
                elif DBG < 41:
                    self.ffn_phase(l, which, src, dst)
                src = self.xs
        P.emit(final_waits=self.out_dmas)
        return nc

    def rows(self, t_ap, r0, n=128):
        return t_ap[r0:r0 + n, :]

    def ffn_phase(self, l, which, src, dst):
        P = self.P
        nc = self.nc
        is_out = dst is self.out
        pre_g = self.prm[which + "_pre_g"]
        post_g = self.prm[which + "_post_g"]
        wgd = self.prm[which + "_w_gate"][l].rearrange("(kc p) f -> p kc f", p=128)
        wud = self.prm[which + "_w_up"][l].rearrange("(kc p) f -> p kc f", p=128)
        wdd = self.prm[which + "_w_down"][l].rearrange("(fc p) d -> p fc d", p=128)
        self.barrier()
        self.sb_off = self.regB
        gpre = self.sb("gpre", [128, D], F32)
        gpost = self.sb("gpost", [128, D], F32)
        xst = [self.sb("xst", [128, D], F32) for _ in range(2)]
        hst = [self.sb("hst", [128, D], BF16) for _ in range(2)]
        hT = [self.sb("hT", [128, 8, TT], BF16) for _ in range(2)]
        sg = [self.sb("sg", [128, TT], BF16) for _ in range(2)]
        uT = self.sb("uT", [128, NFC, TT], BF16)
        junk = self.sb("junk", [128, D], BF16)
        tst = [self.sb("tst", [128, D], F32) for _ in range(2)]
        stat = self.stat
        tag = "%s%d" % (which, l)
        bank = self.bank

        P.dma("pool", gpre[:], pre_g[l].partition_broadcast(128), writes=["gpre"])
        P.dma("pool", gpost[:], post_g[l].partition_broadcast(128), writes=["gpost"])
        if self.w_loaded != (l, which):
            self.ffn_weights(l, which)
        P.op("pool", lambda e: e.tensor_scalar(out=gpost[:], in0=gpost[:], scalar1=0.5, scalar2=None, op0=ALU.mult),
             reads=["gpost"], writes=["gpost"])

        def prologue(t):
            hTt = hT[t % 2]
            for sub in range(4):
                r0 = t * TT + sub * 128
                xb = xst[sub % 2]
                hb = hst[sub % 2]
                xr, hr = "xst%d" % (sub % 2), "hst%d" % (sub % 2)
                P.dma("sp", xb[:], src[r0:r0 + 128, :], reads=["xs_r%d" % (r0 // 128)], writes=[xr])
                sc = (t * 4 + sub) % 8
                ss = stat[:, sc:sc + 1]
                P.op("act", lambda e, xb=xb, ss=ss: e.activation(out=junk[:], in_=xb[:], func=AF.Square, accum_out=ss),
                     reads=[xr], writes=["junk", "ss%d" % sc])
                if DBG == 21:
                    continue
                P.op("act", lambda e, ss=ss: e.activation(out=ss, in_=ss, func=AF.Sqrt, scale=1.0 / D, bias=self.eps),
                     reads=["eps", "ss%d" % sc], writes=["ss%d" % sc])
                P.op("dve", lambda e, ss=ss: e.reciprocal(out=ss, in_=ss), reads=["ss%d" % sc], writes=["ss%d" % sc])
                P.op("dve", lambda e, xb=xb, hb=hb, ss=ss: e.scalar_tensor_tensor(
                    out=hb[:], in0=xb[:], scalar=ss, in1=gpre[:], op0=ALU.mult, op1=ALU.mult),
                    reads=[xr, "ss%d" % sc, "gpre"], writes=[hr])
                if DBG == 22:
                    continue
                for c in range(8):
                    bk = 4 + c // 2
                    pv = bank[bk][:, :].bitcast(BF16)
                    col = (c % 2) * 512 + sub * 128
                    P.op("pe", lambda e, pv=pv, col=col, hb=hb, c=c: e.transpose(
                        out=pv[:, col:col + 128], in_=hb[:, c * 128:(c + 1) * 128], identity=self.ident[:]),
                        reads=[hr, "ident"], writes=["bank%d" % bk])
            if DBG in (21, 22, 23):
                return
            for c in range(8):
                bk = 4 + c // 2
                pv = bank[bk][:, :].bitcast(BF16)
                eng = "act" if bk % 2 == 0 else "dve"
                if eng == "act":
                    fn = lambda e, pv=pv, c=c: e.activation(out=hTt[:, c, :], in_=pv[:, (c % 2) * 512:(c % 2) * 512 + 512], func=AF.Copy)
                else:
                    fn = lambda e, pv=pv, c=c: e.tensor_copy(out=hTt[:, c, :], in_=pv[:, (c % 2) * 512:(c % 2) * 512 + 512])
                P.op(eng, fn, reads=["bank%d" % bk], writes=["hT%d_%d" % (t % 2, c)])

        def gate_up(t, c):
            hTt = hT[t % 2]
            bg, bu = (0, 1) if c % 2 == 0 else (2, 3)
            for kc in range(8):
                P.op("pe", lambda e, kc=kc: e.matmul(bank[bg][:, :], lhsT=self.wg[:, kc, c * 128:(c + 1) * 128],
                                                     rhs=hTt[:, kc, :], start=(kc == 0), stop=(kc == 7)),
                     reads=["wg%d" % c, "hT%d_%d" % (t % 2, kc)], writes=["bank%d" % bg])
            for kc in range(8):
                P.op("pe", lambda e, kc=kc: e.matmul(bank[bu][:, :], lhsT=self.wu[:, kc, c * 128:(c + 1) * 128],
                                                     rhs=hTt[:, kc, :], start=(kc == 0), stop=(kc == 7)),
                     reads=["wu%d" % c, "hT%d_%d" % (t % 2, kc)], writes=["bank%d" % bu])
            sgb = sg[c % 2]
            P.op("act", lambda e: e.activation(out=sgb[:], in_=bank[bg][:, :], func=AF.Silu),
                 reads=["bank%d" % bg], writes=["sg%d" % (c % 2)])
            P.op("dve", lambda e: e.tensor_tensor(out=uT[:, c, :], in0=bank[bu][:, :], in1=sgb[:], op=ALU.mult),
                 reads=["bank%d" % bu, "sg%d" % (c % 2)], writes=["uT%d" % c])

        def down(t, sub):
            b0 = 4 + 2 * (sub % 2)
            r0 = t * TT + sub * 128
            for half in range(2):
                bk = b0 + half
                for c in range(NFC):
                    P.op("pe", lambda e, c=c, bk=bk, half=half: e.matmul(
                        bank[bk][:, :], lhsT=uT[:, c, sub * 128:(sub + 1) * 128],
                        rhs=self.wd[:, c, half * 512:(half + 1) * 512], start=(c == 0), stop=(c == NFC - 1)),
                        reads=["uT%d" % c, "wd%d" % c], writes=["bank%d" % bk])
            sc = 8 + (t * 4 + sub) % 8
            ss2 = [stat[:, sc + 8 * hf:sc + 8 * hf + 1] for hf in range(2)]
            ss = stat[:, sc:sc + 1]
            for hf in range(2):
                P.op("act", lambda e, hf=hf: e.activation(out=junk[:, hf * 512:(hf + 1) * 512], in_=bank[b0 + hf][:, :],
                                                        func=AF.Square, accum_out=ss2[hf]),
                     reads=["bank%d" % (b0 + hf)], writes=["junk", "ssy%d_%d" % (sc, hf)])
            P.op("dve", lambda e: e.tensor_tensor(out=ss, in0=ss2[0], in1=ss2[1], op=ALU.add),
                 reads=["ssy%d_0" % sc, "ssy%d_1" % sc], writes=["ssy%d_0" % sc])
            P.op("act", lambda e: e.activation(out=ss, in_=ss, func=AF.Sqrt, scale=1.0 / D, bias=self.eps),
                 reads=["eps", "ssy%d_0" % sc], writes=["ssy%d_0" % sc])
            P.op("dve", lambda e: e.reciprocal(out=ss, in_=ss), reads=["ssy%d_0" % sc], writes=["ssy%d_0" % sc])
            tb = tst[sub % 2]
            xb = xst[sub % 2]
            tr, xr = "tst%d" % (sub % 2), "xst%d" % (sub % 2)
            P.dma("sp", xb[:], src[r0:r0 + 128, :], reads=["xs_r%d" % (r0 // 128)], writes=[xr])
            for hf in range(2):
                P.op("dve", lambda e, hf=hf: e.scalar_tensor_tensor(
                    out=tb[:, hf * 512:(hf + 1) * 512], in0=bank[b0 + hf][:, :], scalar=ss,
                    in1=gpost[:, hf * 512:(hf + 1) * 512], op0=ALU.mult, op1=ALU.mult),
                    reads=["bank%d" % (b0 + hf), "ssy%d_0" % sc, "gpost"], writes=[tr])
            P.op("dve", lambda e: e.tensor_tensor(out=tb[:], in0=tb[:], in1=xb[:], op=ALU.add),
                 reads=[tr, xr], writes=[tr])
            o = P.dma("sp", dst[r0:r0 + 128, :], tb[:], reads=[tr], writes=["xs_r%d" % (r0 // 128)])
            if is_out:
                self.out_dmas.append(o)

        if DBG == 1:
            return
        prologue(0)
        if DBG in (2, 21, 22, 23, 24, 25):
            return
        for t in range(NT):
            for c in range(NFC):
                gate_up(t, c)
                if c == 10 and t + 1 < NT:
                    prologue(t + 1)
            if DBG == 3:
                return
            for sub in range(4):
                down(t, sub)

    w_loaded = None

    def ffn_weights(self, l, which):
        P = self.P
        wgd = self.prm[which + "_w_gate"][l].rearrange("(kc p) f -> p kc f", p=128)
        wud = self.prm[which + "_w_up"][l].rearrange("(kc p) f -> p kc f", p=128)
        wdd = self.prm[which + "_w_down"][l].rearrange("(fc p) d -> p fc d", p=128)
        amb, P.ambient = P.ambient, []
        groups = [(i * 512, min(FF, (i + 1) * 512)) for i in range((FF + 511) // 512)]
        for gi, (a, b) in enumerate(groups):
            P.dma("pool", self.wg[:, :, a:b], wgd[:, :, a:b], writes=["wg%d" % c for c in range(a // 128, b // 128)])
            P.dma("pool", self.wu[:, :, a:b], wud[:, :, a:b], writes=["wu%d" % c for c in range(a // 128, b // 128)])
        for c0 in range(0, NFC, 2):
            P.dma("pool", self.wd[:, c0:c0 + 2, :], wdd[:, c0:c0 + 2, :], writes=["wd%d" % c0, "wd%d" % (c0 + 1)])
        P.ambient = amb
        self.w_loaded = (l, which)

    def barrier(self):
        self.P.op("pool", lambda e: e.memset(self.stat[:, 62:63], 0.0), writes=["regB"])

    def attn_phase(self, l, src, dst):
        if DBG == 41:
            return
        self.proj_phase(l, src)
        if DBG == 31 or DBG >= 41:
            return
        if self.stages > 3 * l + 2:
            self.ffn_weights(l, "ffn2")
        self.attn_core(l, moba=True)
        if DBG == 32:
            return
        self.attn_core(l, moba=False)
        self.outproj_phase(l, src, dst)

    def prologue_x(self, t, src, gpre, xst, hst, hTt, junk, htag, part="all"):
        P, bank, stat = self.P, self.bank, self.stat
        nh_ = len(hst)
        for sub in range(4):
            r0 = t * TT + sub * 128
            xb, hb = xst[sub % 2], hst[sub % nh_]
            xr, hr = "xst%d" % (sub % 2), "hst%d" % (sub % nh_)
            if part == "tr":
                for c in range(8):
                    bk = 4 + c // 2
                    pv = bank[bk][:, :].bitcast(BF16)
                    col = (c % 2) * 512 + sub * 128
                    P.op("pe", lambda e, pv=pv, col=col, hb=hb, c=c: e.transpose(
                        out=pv[:, col:col + 128], in_=hb[:, c * 128:(c + 1) * 128], identity=self.ident[:]),
                        reads=[hr, "ident"], writes=["bank%d" % bk])
                continue
            P.dma("sp", xb[:], src[r0:r0 + 128, :], reads=["xs_r%d" % (r0 // 128)], writes=[xr])
            sc = (t * 4 + sub) % 8
            ss = stat[:, sc:sc + 1]
            P.op("act", lambda e, xb=xb, ss=ss: e.activation(out=junk[:], in_=xb[:], func=AF.Square, accum_out=ss),
                 reads=[xr], writes=["junk", "ss%d" % sc])
            P.op("act", lambda e, ss=ss: e.activation(out=ss, in_=ss, func=AF.Sqrt, scale=1.0 / D, bias=self.eps),
                 reads=["eps", "ss%d" % sc], writes=["ss%d" % sc])
            P.op("dve", lambda e, ss=ss: e.reciprocal(out=ss, in_=ss), reads=["ss%d" % sc], writes=["ss%d" % sc])
            P.op("dve", lambda e, xb=xb, hb=hb, ss=ss: e.scalar_tensor_tensor(
                out=hb[:], in0=xb[:], scalar=ss, in1=gpre[:], op0=ALU.mult, op1=ALU.mult),
                reads=[xr, "ss%d" % sc, "gpre"], writes=[hr])
            if part == "norm":
                continue
            for c in range(8):
                bk = 4 + c // 2
                pv = bank[bk][:, :].bitcast(BF16)
                col = (c % 2) * 512 + sub * 128
                P.op("pe", lambda e, pv=pv, col=col, hb=hb, c=c: e.transpose(
                    out=pv[:, col:col + 128], in_=hb[:, c * 128:(c + 1) * 128], identity=self.ident[:]),
                    reads=[hr, "ident"], writes=["bank%d" % bk])
        if part == "norm":
            return
        for c in range(8):
            bk = 4 + c // 2
            pv = bank[bk][:, :].bitcast(BF16)
            a = (c % 2) * 512
            if bk % 2 == 0:
                P.op("act", lambda e, pv=pv, c=c, a=a: e.activation(out=hTt[:, c, :], in_=pv[:, a:a + 512], func=AF.Copy),
                     reads=["bank%d" % bk], writes=["%s_%d" % (htag, c)])
            else:
                P.op("dve", lambda e, pv=pv, c=c, a=a: e.tensor_copy(out=hTt[:, c, :], in_=pv[:, a:a + 512]),
                     reads=["bank%d" % bk], writes=["%s_%d" % (htag, c)])

    def epilogue_y(self, b0, r0, idx, src, dst, gpost, xst, tst, junk):
        P, bank, stat = self.P, self.bank, self.stat
        sc = 8 + idx % 8
        ss2 = [stat[:, sc + 8 * hf:sc + 8 * hf + 1] for hf in range(2)]
        ss = stat[:, sc:sc + 1]
        for hf in range(2):
            P.op("act", lambda e, hf=hf: e.activation(out=junk[:, hf * 512:(hf + 1) * 512], in_=bank[b0 + hf][:, :],
                                                    func=AF.Square, accum_out=ss2[hf]),
                 reads=["bank%d" % (b0 + hf)], writes=["junk", "ssy%d_%d" % (sc, hf)])
        P.op("dve", lambda e: e.tensor_tensor(out=ss, in0=ss2[0], in1=ss2[1], op=ALU.add),
             reads=["ssy%d_0" % sc, "ssy%d_1" % sc], writes=["ssy%d_0" % sc])
        P.op("act", lambda e: e.activation(out=ss, in_=ss, func=AF.Sqrt, scale=1.0 / D, bias=self.eps),
             reads=["eps", "ssy%d_0" % sc], writes=["ssy%d_0" % sc])
        P.op("dve", lambda e: e.reciprocal(out=ss, in_=ss), reads=["ssy%d_0" % sc], writes=["ssy%d_0" % sc])
        tb, xb = tst[idx % 2], xst[idx % 2]
        tr, xr = "tst%d" % (idx % 2), "xst%d" % (idx % 2)
        P.dma("sp", xb[:], src[r0:r0 + 128, :], reads=["xs_r%d" % (r0 // 128)], writes=[xr])
        for hf in range(2):
            P.op("dve", lambda e, hf=hf: e.scalar_tensor_tensor(
                out=tb[:, hf * 512:(hf + 1) * 512], in0=bank[b0 + hf][:, :], scalar=ss,
                in1=gpost[:, hf * 512:(hf + 1) * 512], op0=ALU.mult, op1=ALU.mult),
                reads=["bank%d" % (b0 + hf), "ssy%d_0" % sc, "gpost"], writes=[tr])
        P.op("dve", lambda e: e.tensor_tensor(out=tb[:], in0=tb[:], in1=xb[:], op=ALU.add), reads=[tr, xr], writes=[tr])
        o = P.dma("sp", dst[r0:r0 + 128, :], tb[:], reads=[tr], writes=["xs_r%d" % (r0 // 128)])
        if dst is self.out:
            self.out_dmas.append(o)

    def attn_consts(self):
        P = self.P
        NB = S // 256
        self.tri = self.sb("tri", [128, 128], BF16)
        self.btab = self.sb("btab", [128, 12, 32], F32)
        self.regB = self.sb_off
        tmpf = self.sb("tmpf", [128, 128], F32)
        t0 = self.sb("t0", [128, 32], F32)
        poshi = self.sb("poshi", [1, S], BF16)
        poslo = self.sb("poslo", [1, S], BF16)
        onehot = self.sb("onehot", [16, S], BF16)
        slr = [self.sb("slr", [2, S], BF16) for _ in range(4)]
        P.op("pool", lambda e: e.memset(tmpf[:], NEG), writes=["tmpf"])
        P.op("pool", lambda e: e.affine_select(out=tmpf[:], in_=tmpf[:], pattern=[[-1, 128]], compare_op=ALU.is_gt,
                                               fill=0.0, base=0, channel_multiplier=1), reads=["tmpf"], writes=["tmpf"])
        P.op("pool", lambda e: e.tensor_copy(out=self.tri[:], in_=tmpf[:]), reads=["tmpf"], writes=["tri"])
        P.op("pool", lambda e: e.iota(t0[:], pattern=[[-128, 32]], base=384, channel_multiplier=1,
                                      allow_small_or_imprecise_dtypes=True), writes=["t0"])
        self.slopes = [2.0 ** (-(i + 1)) for i in range(MH)] + [2.0 ** (-2 * (i + 1)) for i in range(DH)]
        for hh in range(12):
            P.op("pool", lambda e, hh=hh: e.tensor_scalar(out=self.btab[:, hh, :], in0=t0[:], scalar1=self.slopes[hh],
                                                         scalar2=None, op0=ALU.mult), reads=["t0"], writes=["btab"])
        P.op("pool", lambda e: e.iota(poshi[:], pattern=[[0, S // 512], [256, 2], [0, 256]], base=0, channel_multiplier=0,
                                      allow_small_or_imprecise_dtypes=True), writes=["poshi"])
        P.op("pool", lambda e: e.iota(poslo[:], pattern=[[0, S // 256], [1, 256]], base=0, channel_multiplier=0,
                                      allow_small_or_imprecise_dtypes=True), writes=["poslo"])
        P.op("pool", lambda e: e.memset(onehot[:], 1.0), writes=["onehot"])
        P.op("pool", lambda e: e.affine_select(out=onehot[:], in_=onehot[:], pattern=[[1, NB], [0, 256]], compare_op=ALU.is_equal,
                                               fill=0.0, base=0, channel_multiplier=-1), reads=["onehot"], writes=["onehot"])
        for h in range(MH):
            P.dma("sp", self.QA[h, 80:81, :], poshi[:], reads=["poshi"])
            P.dma("sp", self.QA[h, 81:82, :], poslo[:], reads=["poslo"])
            P.dma("sp", self.KA[h, 64:80, :], onehot[:], reads=["onehot"])
        for h in range(DH):
            for m in range(2):
                P.dma("sp", self.QD[h, m, 64:65, :], poshi[:], reads=["poshi"])
                P.dma("sp", self.QD[h, m, 65:66, :], poslo[:], reads=["poslo"])
        for hh in range(12):
            sl = slr[hh % 4]
            P.op("pool", lambda e, sl=sl, hh=hh: e.memset(sl[:], -8.0 * self.slopes[hh]), writes=["slr%d" % (hh % 4)])
            if hh < MH:
                P.dma("sp", self.KA[hh, 80:82, :], sl[:], reads=["slr%d" % (hh % 4)])
            else:
                for m in range(2):
                    P.dma("sp", self.KD[hh - MH, m, 64:66, :], sl[:], reads=["slr%d" % (hh % 4)])

    def proj_phase(self, l, src):
        P, bank, stat = self.P, self.bank, self.stat
        NB = S // 256
        self.barrier()
        self.sb_off = self.regB
        gpre = self.sb("gpre", [128, D], F32)
        xst = [self.sb("xst", [128, D], F32) for _ in range(2)]
        hst = [self.sb("hst", [128, D], BF16) for _ in range(4)]
        hT = [self.sb("hT", [128, 8, TT], BF16) for _ in range(2)]
        junk = self.sb("junk", [128, D], BF16)
        qst = [self.sb("qst", [128, TT], BF16) for _ in range(NQST)]
        vms = [self.sb("vms", [128, MH, 65], BF16) for _ in range(2)]
        vds = [self.sb("vds", [128, DH, 129], BF16) for _ in range(2)]
        kmf = self.sb("kmf", [128, 4, 16], F32)
        kmb = self.sb("kmb", [128, 4, 32], BF16)
        maskc = self.sb("maskc", [128, 16, 128], F32)
        mt1 = self.sb("mt1", [128, 16, 128], F32)
        gsb = [self.sb("gsb", [128, 128], F32) for _ in range(2)]
        mx8 = [self.sb("mx8", [128, MH, 8], F32) for _ in range(2)]
        selb = [self.sb("selb", [128, 128], BF16) for _ in range(4)]
        selT = [self.sb("selT", [128, TT], BF16) for _ in range(2)]
        win = self.nc.alloc_sbuf_tensor_at("win_%d" % l, [128, 8, INW], BF16, offset=SB_BASE)
        wind = self.prm["w_in"][l].rearrange("(kc p) f -> p kc f", p=128)
        allw = ["wg%d" % c for c in range(NFC)] + ["wu%d" % c for c in range(NFC)]
        for g in range(6):
            P.dma("pool", win[:, :, g * 512:(g + 1) * 512], wind[:, :, g * 512:(g + 1) * 512],
                  writes=(allw if g == 0 else []) + ["win%d" % g])
        P.dma("pool", gpre[:], self.prm["mix_pre_g"][l].partition_broadcast(128), writes=["gpre"])
        P.op("pool", lambda e: e.memset(kmf[:], 0.0), writes=["kmf"])
        P.op("pool", lambda e: e.memset(kmb[:], 0.0), writes=["kmb"])
        for b_ in range(2):
            P.op("pool", lambda e, b_=b_: e.memset(vms[b_][:, :, 64:65], 1.0), writes=["vms%d" % b_])
            P.op("pool", lambda e, b_=b_: e.memset(vds[b_][:, :, 128:129], 1.0), writes=["vds%d" % b_])
        P.op("pool", lambda e: e.iota(mt1[:], pattern=[[-1, 16], [0, MH], [1, 16]], base=0, channel_multiplier=0,
                                      allow_small_or_imprecise_dtypes=True), writes=["mt1"])
        P.op("pool", lambda e: e.tensor_scalar(out=maskc[:], in0=mt1[:], scalar1=0.0, scalar2=-1e30, op0=ALU.is_gt, op1=ALU.mult),
             reads=["mt1"], writes=["maskc"])
        P.op("pool", lambda e: e.tensor_scalar(out=mt1[:], in0=mt1[:], scalar1=0.0, scalar2=1e30, op0=ALU.is_equal, op1=ALU.mult),
             reads=["mt1", "maskc"], writes=["mt1"])
        P.op("pool", lambda e: e.tensor_tensor(out=maskc[:], in0=maskc[:], in1=mt1[:], op=ALU.add),
             reads=["mt1", "maskc"], writes=["maskc"])
        wr = ["wg0"]
        cnt = {"pb": 0, "q": 0}
        if DBG == 42:
            return

        def fm_chunk(t, oc):
            bk = cnt["pb"] % 2
            cnt["pb"] += 1
            for kc in range(8):
                P.op("pe", lambda e, kc=kc, bk=bk: e.matmul(bank[bk][:, :], lhsT=win[:, kc, oc * 128:(oc + 1) * 128],
                                                           rhs=hT[t % 2][:, kc, :], start=(kc == 0), stop=(kc == 7)),
                     reads=["win%d" % (oc // 4), "hTp%d_%d" % (t % 2, kc)] + wr, writes=["bank%d" % bk])
            return bk

        def evac_q(bk):
            qi = cnt["q"] % NQST
            cnt["q"] += 1
            P.op("act", lambda e: e.activation(out=qst[qi][:], in_=bank[bk][:, :], func=AF.Copy),
                 reads=["bank%d" % bk], writes=["qst%d" % qi])
            return qi

        cols = lambda t: slice(t * TT, (t + 1) * TT)
        self.prologue_x(0, src, gpre, xst, hst, hT[0], junk, "hTp0")
        for t in range(NT):
            if t + 1 < NT:
                self.prologue_x(t + 1, src, gpre, xst, hst, hT[(t + 1) % 2], junk, "hTp%d" % ((t + 1) % 2), part="norm")
            for oc in range(4, 8):
                bk = fm_chunk(t, oc)
                qi = evac_q(bk)
                P.op("dve", lambda e, bk=bk, oc=oc, t=t: e.tensor_reduce(
                    out=kmf[:, oc - 4, 2 * t:2 * t + 2], in_=bank[bk][:, :].rearrange("p (a b) -> p a b", a=2),
                    axis=AX.X, op=ALU.add), reads=["bank%d" % bk, "kmf"], writes=["kmf"])
                for hh in range(2):
                    P.dma("sp", self.KA[2 * (oc - 4) + hh, 0:64, cols(t)], qst[qi][hh * 64:(hh + 1) * 64, :],
                          reads=["qst%d" % qi], writes=["KA"])
            for hh in range(2):
                P.op("dve", lambda e, hh=hh: e.tensor_scalar(
                    out=kmb[hh * 64:(hh + 1) * 64, :, hh * 16:(hh + 1) * 16], in0=kmf[hh * 64:(hh + 1) * 64, :, :],
                    scalar1=1.0 / 256, scalar2=None, op0=ALU.mult), reads=["kmf"], writes=["kmb"])
            for oc in range(0, 4):
                bk = fm_chunk(t, oc)
                qi = evac_q(bk)
                for hh in range(2):
                    P.dma("sp", self.QA[2 * oc + hh, 0:64, cols(t)], qst[qi][hh * 64:(hh + 1) * 64, :],
                          reads=["qst%d" % qi], writes=["QA"])
                for s_ in range(4):
                    P.op("pe", lambda e, s_=s_, qi=qi, oc=oc: e.matmul(
                        bank[3][:, s_ * 128 + oc * 32:s_ * 128 + oc * 32 + 32],
                        lhsT=qst[qi][:, s_ * 128:(s_ + 1) * 128],
                        rhs=kmb[:, oc, :], start=True, stop=True, skip_group_check=True),
                        reads=["qst%d" % qi, "kmb"], writes=["bank3"])
            pvT = bank[2][:, :].bitcast(BF16)
            for s_ in range(4):
                cur = 2 * t + s_ // 2
                gb, mb, sbb = gsb[s_ % 2], mx8[s_ % 2], selb[s_]
                gr, mr, sr = "gsb%d" % (s_ % 2), "mx8%d" % (s_ % 2), "selb%d" % s_
                P.op("dve", lambda e, s_=s_, cur=cur, gb=gb: e.tensor_tensor(
                    out=gb[:], in0=bank[3][:, s_ * 128:(s_ + 1) * 128], in1=maskc[:, cur, :], op=ALU.add),
                    reads=["bank3", "maskc"], writes=[gr])
                for h in range(MH):
                    P.op("dve", lambda e, h=h, gb=gb, mb=mb: e.max(out=mb[:, h, :], in_=gb[:, h * 16:(h + 1) * 16]),
                         reads=[gr], writes=[mr])
                for h in range(MH):
                    P.op("dve", lambda e, h=h, gb=gb, mb=mb, sbb=sbb: e.tensor_scalar(
                        out=sbb[:, h * 16:(h + 1) * 16], in0=gb[:, h * 16:(h + 1) * 16], scalar1=mb[:, h, 3:4], scalar2=NEG,
                        op0=ALU.is_lt, op1=ALU.mult), reads=[gr, mr], writes=[sr])

            def sel_tail(t=t, pvT=pvT):
                for s_ in range(4):
                    sbb, sr = selb[s_], "selb%d" % s_
                    P.op("pe", lambda e, s_=s_, sbb=sbb: e.transpose(out=pvT[:, s_ * 128:(s_ + 1) * 128], in_=sbb[:], identity=self.ident[:]),
                         reads=[sr, "ident"], writes=["bank2"])
                sT = selT[t % 2]
                P.op("act", lambda e, sT=sT: e.activation(out=sT[:], in_=pvT[:, 0:512], func=AF.Copy),
                     reads=["bank2"], writes=["selT%d" % (t % 2)])
                for h in range(MH):
                    P.dma("sp", self.QA[h, 64:80, cols(t)], sT[h * 16:(h + 1) * 16, :], reads=["selT%d" % (t % 2)], writes=["QA"])
            if DBG == 44:
                continue
            for oc in range(12, 20):
                bk = fm_chunk(t, oc)
                qi = evac_q(bk)
                dstT = self.QD if oc < 16 else self.KD
                hd = (oc - 12) % 4
                for m in range(2):
                    P.dma("sp", dstT[hd, m, 0:64, cols(t)], qst[qi][m * 64:(m + 1) * 64, :],
                          reads=["qst%d" % qi], writes=["QD"])
            if DBG == 45:
                continue
            for s_ in range(4):
                r0 = t * TT + s_ * 128
                for which in range(2):
                    bk = cnt["pb"] % 2
                    cnt["pb"] += 1
                    c0 = 1024 if which == 0 else 2560
                    for kc in range(8):
                        P.op("pe", lambda e, kc=kc, bk=bk, c0=c0, s_=s_, t=t: e.matmul(
                            bank[bk][:, :], lhsT=hT[t % 2][:, kc, s_ * 128:(s_ + 1) * 128], rhs=win[:, kc, c0:c0 + 512],
                            start=(kc == 0), stop=(kc == 7)),
                            reads=["win%d" % (c0 // 512), "hTp%d_%d" % (t % 2, kc)] + wr, writes=["bank%d" % bk])
                    if which == 0:
                        vb = vms[s_ % 2]
                        P.op("act", lambda e, bk=bk, vb=vb: e.activation(
                            out=vb[:, :, 0:64], in_=bank[bk][:, :].rearrange("p (h d) -> p h d", h=MH), func=AF.Copy),
                            reads=["bank%d" % bk], writes=["vms%d" % (s_ % 2)])
                        P.dma("sp", self.VM[r0:r0 + 128, :], vb[:].rearrange("p h d -> p (h d)"),
                              reads=["vms%d" % (s_ % 2)], writes=["VM"])
                    else:
                        vb = vds[s_ % 2]
                        P.op("dve", lambda e, bk=bk, vb=vb: e.tensor_copy(
                            out=vb[:, :, 0:128], in_=bank[bk][:, :].rearrange("p (h d) -> p h d", h=DH)),
                            reads=["bank%d" % bk], writes=["vds%d" % (s_ % 2)])
                        P.dma("sp", self.VD[r0:r0 + 128, :], vb[:].rearrange("p h d -> p (h d)"),
                              reads=["vds%d" % (s_ % 2)], writes=["VD"])
            sel_tail()
            if t + 1 < NT:
                self.prologue_x(t + 1, src, gpre, xst, hst, hT[(t + 1) % 2], junk, "hTp%d" % ((t + 1) % 2), part="tr")

    def attn_core(self, l, moba):
        P, bank, stat = self.P, self.bank, self.stat
        NKT = S // 128
        self.barrier()
        self.sb_off = self.regB
        nh = MH if moba else DH
        nm = 1 if moba else 2
        K = 82 if moba else 66
        dv = 64 if moba else 128
        vw = nh * (dv + 1)
        Vs = self.sb("Vs", [128, NKT, vw], BF16)
        Vd = (self.VM if moba else self.VD).rearrange("(i p) c -> p i c", p=128)
        step = max(1, NKT // 4)
        for i0 in range(0, NKT, step):
            P.dma("sp", Vs[:, i0:i0 + step, :], Vd[:, i0:i0 + step, :], reads=["VM", "VD"], writes=["Vs"])
        nqb = 2 if moba else 1
        Qb = [[self.sb("Qb", [128, S], BF16) for m in range(nm)] for _ in range(nqb)]
        Kb = [[self.sb("Kb", [128, S], BF16) for m in range(nm)] for _ in range(nqb)]
        for a_ in range(nqb):
            for m in range(nm):
                P.op("pool", lambda e, a_=a_, m=m: e.memset(Qb[a_][m][:], 0.0), writes=["Qb%d_%d" % (a_, m), "QKz"])
                P.op("pool", lambda e, a_=a_, m=m: e.memset(Kb[a_][m][:], 0.0), writes=["Kb%d_%d" % (a_, m), "QKz"])
        npt = 4 if moba else 3
        pt = [self.sb("pt", [128, 512], BF16) for _ in range(npt)]
        ostg = [self.sb("ostg", [128, 4, dv], BF16) for _ in range(2)]
        rz = self.sb("rz", [128, 16], F32)
        if not moba:
            lams = self.sb("lams", [128, 8], F32)
            subg = self.sb("subg", [128, 128], F32)
            t1off = self.sb_off
            t1 = [self.sb("t1", [128, 128], F32) for _ in range(2)]
            lamt = self.sb("lamt", [128, 4, 64], F32, off=t1off)
            asb = [self.sb("asb", [128, 128], F32) for _ in range(4)]
            junk = self.sb("junk", [128, 128], BF16)
            lam_init = 0.8 - 0.6 * math.exp(-0.3 * l)
            for i, nm_ in enumerate(["lambda_q1", "lambda_k1", "lambda_q2", "lambda_k2"]):
                P.dma("sp", lamt[:, i, :], self.prm[nm_][l].partition_broadcast(128), writes=["lamt"])
            P.dma("sp", subg[:], self.prm["subln_g"][l].partition_broadcast(128), writes=["subg"])
            P.op("dve", lambda e: e.tensor_scalar(out=subg[:], in0=subg[:], scalar1=1.0 - lam_init, scalar2=None, op0=ALU.mult),
                 reads=["subg"], writes=["subg"])
            for i in range(2):
                P.op("dve", lambda e, i=i: e.tensor_tensor(out=lamt[:, 2 * i, :], in0=lamt[:, 2 * i, :], in1=lamt[:, 2 * i + 1, :], op=ALU.mult),
                     reads=["lamt"], writes=["lamt"])
                P.op("dve", lambda e, i=i: e.tensor_reduce(out=lams[:, i:i + 1], in_=lamt[:, 2 * i, :], axis=AX.X, op=ALU.add),
                     reads=["lamt"], writes=["lams"])
                P.op("act", lambda e, i=i: e.activation(out=lams[:, i:i + 1], in_=lams[:, i:i + 1], func=AF.Exp),
                     reads=["lams"], writes=["lams"])
            P.op("dve", lambda e: e.tensor_tensor(out=lams[:, 2:3], in0=lams[:, 1:2], in1=lams[:, 0:1], op=ALU.subtract),
                 reads=["lams"], writes=["lams"])
            P.op("dve", lambda e: e.tensor_scalar(out=lams[:, 2:3], in0=lams[:, 2:3], scalar1=-lam_init, scalar2=None, op0=ALU.add),
                 reads=["lams"], writes=["lams"])
            neglam = lams[:, 2:3]
        sbanks = [0, 1, 2] if moba else [0, 1]
        osets = [[4], [5], [6], [7]] if moba else [[2, 3, 4], [5, 6, 7]]
        NQ = S // 512
        units = []
        for h in range(nh):
            for j in range(NQ):
                oset = osets[(h * NQ + j) % len(osets)]
                for i in range(4 * j + 4):
                    for m in range(nm):
                        units.append(dict(h=h, j=j, i=i, m=m, oset=oset, first=(j == 0 and i == 0 and m == 0),
                                          last=(i == 4 * j + 3 and m == nm - 1)))
        started = {}

        def acc(u, s_, m):
            if moba:
                return u["oset"][0], s_ * 65
            a = s_ * 2 + m
            return u["oset"][a // 3], (a % 3) * 129

        def load_head(h):
            qb = h % nqb
            for m in range(nm):
                qsrc = self.QA[h] if moba else self.QD[h, m]
                ksrc = self.KA[h] if moba else self.KD[h, m]
                P.dma("sp", Qb[qb][m][0:K, :], qsrc, reads=["QA", "QD"], writes=["Qb%d_%d" % (qb, m)])
                P.dma("sp", Kb[qb][m][0:K, :], ksrc, reads=["KA", "KD"], writes=["Kb%d_%d" % (qb, m)])

        def stageA(n):
            u = units[n]
            h, j, i, m = u["h"], u["j"], u["i"], u["m"]
            if u["first"]:
                if h == 0 or nqb == 1:
                    load_head(h)
                if nqb == 2 and h + 1 < nh:
                    load_head(h + 1)
            qb = h % nqb
            hh = h if moba else MH + h
            d = i - 4 * j
            qlo = 128 * d if d > 0 else 0
            sb_ = sbanks[n % len(sbanks)]
            pb = n % npt
            P.op("pe", lambda e: e.matmul(
                bank[sb_][:, qlo:512], lhsT=Kb[qb][m][0:K, i * 128:(i + 1) * 128],
                rhs=Qb[qb][m][0:K, j * 512 + qlo:(j + 1) * 512], start=True, stop=(d < 0)),
                reads=["Kb%d_%d" % (qb, m), "Qb%d_%d" % (qb, m), "QKz"], writes=["bank%d" % sb_])
            if d >= 0:
                P.op("pe", lambda e: e.matmul(
                    bank[sb_][:, qlo:qlo + 128], lhsT=self.ident[:], rhs=self.tri[:], start=False, stop=True),
                    reads=["ident", "tri"], writes=["bank%d" % sb_])
            P.op("act", lambda e: e.activation(
                out=pt[pb][:, qlo:512], in_=bank[sb_][:, qlo:512], func=AF.Exp, scale=0.125,
                bias=self.btab[:, hh, 4 * j - i + 3:4 * j - i + 4]),
                reads=["bank%d" % sb_, "btab"], writes=["pt%d" % pb])

        def stageB(n):
            u = units[n]
            h, j, i, m = u["h"], u["j"], u["i"], u["m"]
            d = i - 4 * j
            pb = n % npt
            key = (h, j)
            st_set = started.setdefault(key, set())
            for s_ in range(max(d, 0), 4):
                bk, c0 = acc(u, s_, m)
                st = bk not in st_set
                st_set.add(bk)
                P.op("pe", lambda e, bk=bk, c0=c0, s_=s_, st=st: e.matmul(
                    bank[bk][:, c0:c0 + dv + 1], lhsT=pt[pb][:, s_ * 128:(s_ + 1) * 128],
                    rhs=Vs[:, i, h * (dv + 1):(h + 1) * (dv + 1)], start=st, stop=(i == 4 * j + s_),
                    skip_group_check=True),
                    reads=["pt%d" % pb, "Vs"], writes=["bank%d" % bk])
            if u["last"]:
                epilogue(u)

        ecount = {"n": 0}
        pending = []

        def epilogue(u):
            h, j = u["h"], u["j"]
            en = ecount["n"]
            ecount["n"] += 1
            og = ostg[en % 2]
            orr = "ostg%d" % (en % 2)
            if moba:
                bk = u["oset"][0]
                ov = bank[bk][:, 0:260].rearrange("p (s c) -> p s c", s=4)
                P.op("dve", lambda e: e.reciprocal(out=rz[:, 0:4], in_=ov[:, :, 64]), reads=["bank%d" % bk], writes=["rz"])
                for s_ in range(4):
                    P.op("dve", lambda e, s_=s_: e.tensor_scalar(
                        out=og[:, s_, :], in0=ov[:, s_, 0:64], scalar1=rz[:, s_:s_ + 1], scalar2=None, op0=ALU.mult),
                        reads=["bank%d" % bk, "rz"], writes=[orr])
                P.dma("sp", self.MIX[j * 512:(j + 1) * 512, h * 64:(h + 1) * 64].rearrange("(s p) c -> p s c", p=128),
                      og[:], reads=[orr], writes=["MIX"])
                return
            for s_ in range(4):
                b1, c1 = acc(u, s_, 0)
                b2, c2 = acc(u, s_, 1)
                tt, aa = t1[s_ % 2], asb[s_]
                tr, ar = "t1%d" % (s_ % 2), "asb%d" % s_
                r1, r2, ssq = rz[:, 4 * s_:4 * s_ + 1], rz[:, 4 * s_ + 1:4 * s_ + 2], rz[:, 4 * s_ + 2:4 * s_ + 3]
                rr = "rz%d" % s_
                P.op("dve", lambda e, b1=b1, c1=c1, r1=r1: e.reciprocal(out=r1, in_=bank[b1][:, c1 + 128:c1 + 129]),
                     reads=["bank%d" % b1], writes=[rr])
                P.op("dve", lambda e, b2=b2, c2=c2, r2=r2: e.reciprocal(out=r2, in_=bank[b2][:, c2 + 128:c2 + 129]),
                     reads=["bank%d" % b2, rr], writes=[rr])
                P.op("dve", lambda e, r2=r2: e.tensor_tensor(out=r2, in0=r2, in1=neglam, op=ALU.mult),
                     reads=[rr, "lams"], writes=[rr])
                P.op("dve", lambda e, b1=b1, c1=c1, r1=r1, tt=tt: e.tensor_scalar(
                    out=tt[:], in0=bank[b1][:, c1:c1 + 128], scalar1=r1, scalar2=None, op0=ALU.mult),
                    reads=["bank%d" % b1, rr], writes=[tr])
                P.op("dve", lambda e, b2=b2, c2=c2, r2=r2, tt=tt, aa=aa: e.scalar_tensor_tensor(
                    out=aa[:], in0=bank[b2][:, c2:c2 + 128], scalar=r2, in1=tt[:], op0=ALU.mult, op1=ALU.add),
                    reads=["bank%d" % b2, rr, tr], writes=[ar])
                P.op("dve", lambda e, aa=aa, ssq=ssq: e.scalar_tensor_tensor(
                    out=junk[:], in0=aa[:], scalar=1.0, in1=aa[:], op0=ALU.mult, op1=ALU.mult, accum_out=ssq),
                    reads=[ar, rr], writes=["junkd", rr])

            def part2(h=h, j=j, og=og, orr=orr):
                ssq4 = rz[:, 2:16:4]
                rrs = ["rz%d" % s_ for s_ in range(4)]
                P.op("act", lambda e: e.activation(out=ssq4, in_=ssq4, func=AF.Sqrt, scale=1.0 / 128, bias=self.eps),
                     reads=["eps"] + rrs, writes=rrs)
                P.op("dve", lambda e: e.reciprocal(out=ssq4, in_=ssq4), reads=rrs, writes=rrs)
                for s_ in range(4):
                    aa, ar = asb[s_], "asb%d" % s_
                    ssq = rz[:, 4 * s_ + 2:4 * s_ + 3]
                    P.op("dve", lambda e, aa=aa, ssq=ssq, s_=s_: e.scalar_tensor_tensor(
                        out=og[:, s_, :], in0=aa[:], scalar=ssq, in1=subg[:], op0=ALU.mult, op1=ALU.mult),
                        reads=[ar, "rz%d" % s_, "subg"], writes=[orr])
                P.dma("sp", self.MIX[j * 512:(j + 1) * 512, 512 + h * 128:512 + (h + 1) * 128].rearrange("(s p) c -> p s c", p=128),
                      og[:], reads=[orr], writes=["MIX"])
            pending.append([3, part2])

        LA = 2 if moba else 1
        for n in range(min(LA, len(units))):
            stageA(n)
        for n in range(len(units)):
            if n + LA < len(units):
                stageA(n + LA)
            for pd in list(pending):
                pd[0] -= 1
                if pd[0] <= 0:
                    pending.remove(pd)
                    pd[1]()
            stageB(n)
        for pd in pending:
            pd[1]()


    def outproj_phase(self, l, src, dst):
        P, bank, stat = self.P, self.bank, self.stat
        self.barrier()
        self.sb_off = self.regB
        wo = self.sb("wo", [128, 8, D], BF16)
        gpost = self.sb("gpost", [128, D], F32)
        mst = [self.sb("mst", [128, D], BF16) for _ in range(3)]
        mT = [self.sb("mT", [128, 8, 128], BF16) for _ in range(2)]
        xst = [self.sb("xst", [128, D], F32) for _ in range(2)]
        tst = [self.sb("tst", [128, D], F32) for _ in range(2)]
        junk = self.sb("junk", [128, D], BF16)
        wod = self.prm["w_out"][l].rearrange("(kc p) f -> p kc f", p=128)
        for g in range(2):
            P.dma("pool", wo[:, :, g * 512:(g + 1) * 512], wod[:, :, g * 512:(g + 1) * 512], writes=["wo%d" % g])
        P.dma("pool", gpost[:], self.prm["mix_post_g"][l].partition_broadcast(128), writes=["gpost"])
        def load_mix(r):
            P.dma("sp", mst[r % 3][:], self.MIX[r * 128:(r + 1) * 128, :], reads=["MIX"], writes=["mst%d" % (r % 3)])

        load_mix(0)
        load_mix(1)
        for r in range(S // 128):
            r0 = r * 128
            mb, mtb = mst[r % 3], mT[r % 2]
            if r + 2 < S // 128:
                load_mix(r + 2)
            tb_ = r % 2
            pv = bank[tb_][:, :].bitcast(BF16)
            for c in range(8):
                P.op("pe", lambda e, pv=pv, c=c, mb=mb: e.transpose(out=pv[:, c * 128:(c + 1) * 128], in_=mb[:, c * 128:(c + 1) * 128],
                                                                  identity=self.ident[:]),
                     reads=["mst%d" % (r % 3), "ident"], writes=["bank%d" % tb_])
            if tb_ == 0:
                P.op("act", lambda e, pv=pv, mtb=mtb: e.activation(out=mtb[:].rearrange("p c t -> p (c t)"), in_=pv[:, :], func=AF.Copy),
                     reads=["bank%d" % tb_], writes=["mT%d" % (r % 2)])
            else:
                P.op("dve", lambda e, pv=pv, mtb=mtb: e.tensor_copy(out=mtb[:].rearrange("p c t -> p (c t)"), in_=pv[:, :]),
                     reads=["bank%d" % tb_], writes=["mT%d" % (r % 2)])
            b0 = 4 + 2 * (r % 2)
            for hf in range(2):
                for c in range(8):
                    P.op("pe", lambda e, c=c, hf=hf, mtb=mtb, b0=b0: e.matmul(bank[b0 + hf][:, :], lhsT=mtb[:, c, :],
                                                                     rhs=wo[:, c, hf * 512:(hf + 1) * 512], start=(c == 0), stop=(c == 7)),
                         reads=["mT%d" % (r % 2), "wo%d" % hf], writes=["bank%d" % (b0 + hf)])
            self.epilogue_y(b0, r0, r, src, dst, gpost, xst, tst, junk)


_NC_CACHE = {}


def get_nc(stages):
    if stages not in _NC_CACHE:
        _NC_CACHE[stages] = Builder(stages).build()
    return _NC_CACHE[stages]


STAGES = 6
DBG = 0


def kernel(**inputs):
    nc = get_nc(STAGES)
    x = np.ascontiguousarray(inputs["x"], dtype=np.float32)
    shared = {k: np.ascontiguousarray(v, dtype=np.float32) for k, v in inputs.items() if k != "x"}
    in_maps = []
    for b in range(8):
        m = dict(shared)
        m["x"] = x[b]
        in_maps.append(m)
    res = run_bass_kernel_spmd(nc, in_maps, core_ids=list(range(8)))
    return np.stack([res.results[b]["out"] for b in range(8)], axis=0)
```
